# Optimizing a Trainium2 kernel written in Bass

```python
import math
import jax, jax.numpy as jnp
from jax import lax
import numpy as np

D_MODEL = 1024
BATCH = 8
SEQ = 4096
DEPTH = 4

GRID_W = 64
CTX_LEN = 256
N_MIXERS = 3
EPS = 1e-6
F32 = jnp.float32

D_RNN = 1024
RNN_BLOCKS = 8
RNN_BW = D_RNN // RNN_BLOCKS
RNN_CONV = 4
LRU_C = 8.0

N_HEADS = 16
N_KV_HEADS = 4
HEAD_DIM = 64
WINDOW = 128
ATTN_BLOCK = 128
ROPE_BASE = 10000.0

HYENA_ORDER = 2
HYENA_CONV = 3
FILTER_BANDS = 16
FILTER_EMB = 1 + 2 * FILTER_BANDS
FILTER_WIDTH = 64
DECAY_TARGET = 1e-2
FAST_DECAY_PCT = 0.3
SLOW_DECAY_PCT = 1.5

D_FF = 2816
N_EXPERTS = 8
TOP_K = 2
D_FF_EXPERT = 3584

kernel_name = "hybrid_flow_backbone_rglru_swa_hyena_moe"


def _rmsnorm(x, g):
    x32 = x.astype(F32)
    y = x32 * lax.rsqrt(jnp.mean(x32 * x32, axis=-1, keepdims=True) + EPS)
    return (y * g.astype(F32)).astype(x.dtype)


def _ada_norm(x, g, shift, scale):
    return _rmsnorm(x, g) * (1 + scale) + shift


def _dwconv(x, w, b):
    k = w.shape[0]
    y = lax.conv_general_dilated(
        x, w[:, None, :].astype(x.dtype), window_strides=(1,),
        padding=[(k // 2, k - 1 - k // 2)],
        dimension_numbers=("NWC", "WIO", "NWC"), feature_group_count=x.shape[-1])
    return y + b.astype(x.dtype)


def _axial_rope_tables(rows):
    q = HEAD_DIM // 4
    inv_freq = ROPE_BASE ** (-jnp.arange(q, dtype=F32) / q)
    row = jnp.repeat(jnp.arange(rows, dtype=F32), GRID_W)
    col = jnp.tile(jnp.arange(GRID_W, dtype=F32), rows)
    ang = jnp.concatenate([row[:, None] * inv_freq, col[:, None] * inv_freq], axis=-1)
    return jnp.cos(ang), jnp.sin(ang)


def _apply_rope(x, cos, sin):
    q = HEAD_DIM // 4
    x32 = x.astype(F32)

    def rot(xa, c, s):
        c = c[None, :, None, :]
        s = s[None, :, None, :]
        x1, x2 = xa[..., :q], xa[..., q:]
        return jnp.concatenate([x1 * c - x2 * s, x2 * c + x1 * s], axis=-1)

    out = jnp.concatenate([rot(x32[..., :2 * q], cos[:, :q], sin[:, :q]),
                           rot(x32[..., 2 * q:], cos[:, q:], sin[:, q:])], axis=-1)
    return out.astype(x.dtype)


def _lin_scan(a, b, h0, reverse):
    def combine(e1, e2):
        a1, b1 = e1
        a2, b2 = e2
        return a1 * a2, a2 * b1 + b2
    a_cum, b_cum = lax.associative_scan(combine, (a, b), reverse=reverse, axis=1)
    return a_cum * h0[:, None, :] + b_cum


def _rglru_coeffs(u, w_a, b_a, w_x, b_x, lam):
    ub = u.reshape(u.shape[:-1] + (RNN_BLOCKS, RNN_BW))
    r = jax.nn.sigmoid(jnp.einsum("blhi,hij->blhj", ub, w_a.astype(F32)).reshape(u.shape) + b_a.astype(F32))
    i = jax.nn.sigmoid(jnp.einsum("blhi,hij->blhj", ub, w_x.astype(F32)).reshape(u.shape) + b_x.astype(F32))
    log_a = -LRU_C * r * jax.nn.softplus(-lam.astype(F32))
    return jnp.exp(log_a), jnp.sqrt(-jnp.expm1(2.0 * log_a)) * (i * u)


def _rglru_mixer(h_ctx, h_lat, w_in, conv_w, conv_b, w_a, b_a, w_x, b_x, lam, w_out):
    def branches(h):
        gate, u = jnp.split(h @ w_in, 2, axis=-1)
        return gate, _dwconv(u, conv_w, conv_b).astype(F32)
    gate_c, u_c = branches(h_ctx)
    gate_l, u_l = branches(h_lat)
    h_zero = jnp.zeros((h_ctx.shape[0], D_RNN), F32)
    y_c = jnp.zeros_like(u_c)
    y_l = jnp.zeros_like(u_l)
    for d, reverse in ((0, False), (1, True)):
        a_c, b_c = _rglru_coeffs(u_c, w_a[d], b_a[d], w_x[d], b_x[d], lam[d])
        hs_c = _lin_scan(a_c, b_c, h_zero, reverse)
        h_ctx_final = hs_c[:, 0] if reverse else hs_c[:, -1]
        a_l, b_l = _rglru_coeffs(u_l, w_a[d], b_a[d], w_x[d], b_x[d], lam[d])
        y_l = y_l + _lin_scan(a_l, b_l, h_ctx_final, reverse)
        y_c = y_c + hs_c
    out_c = (y_c.astype(h_ctx.dtype) * jax.nn.gelu(gate_c)) @ w_out
    out_l = (y_l.astype(h_lat.dtype) * jax.nn.gelu(gate_l)) @ w_out
    return out_c, out_l


def _sink_softmax(s, sink):
    sk = jnp.broadcast_to(sink[None, :, :, None, None], s.shape[:-1] + (1,))
    p = jax.nn.softmax(jnp.concatenate([s, sk], axis=-1), axis=-1)
    return p[..., :-1]


def _swa_mixer(h_ctx, h_lat, rope_cos, rope_sin, w_qkv, sinks, w_o):
    B, L, _ = h_lat.shape
    G = N_HEADS // N_KV_HEADS
    scale = HEAD_DIM ** -0.5
    sink = sinks.astype(F32).reshape(N_KV_HEADS, G)

    def qkv(h):
        q, k, v = jnp.split(h @ w_qkv, [N_HEADS * HEAD_DIM, (N_HEADS + N_KV_HEADS) * HEAD_DIM], axis=-1)
        n = h.shape[1]
        return (q.reshape(B, n, N_HEADS, HEAD_DIM), k.reshape(B, n, N_KV_HEADS, HEAD_DIM),
                v.reshape(B, n, N_KV_HEADS, HEAD_DIM))

    q_c, k_c, v_c = qkv(h_ctx)
    q_c = q_c.reshape(B, -1, N_KV_HEADS, G, HEAD_DIM)
    q_l, k_l, v_l = qkv(h_lat)
    q_l = _apply_rope(q_l, rope_cos, rope_sin).reshape(B, L, N_KV_HEADS, G, HEAD_DIM)
    k_l = _apply_rope(k_l, rope_cos, rope_sin)

    s_c = jnp.einsum("bqkgd,bskd->bkgqs", q_c, k_c).astype(F32) * scale
    p_c = _sink_softmax(s_c, sink).astype(v_c.dtype)
    o_c = jnp.einsum("bkgqs,bskd->bqkgd", p_c, v_c).reshape(B, -1, N_HEADS * HEAD_DIM)

    pad = ((0, 0), (ATTN_BLOCK, ATTN_BLOCK), (0, 0), (0, 0))
    k_pad = jnp.pad(k_l, pad)
    v_pad = jnp.pad(v_l, pad)
    offs_q = jnp.arange(ATTN_BLOCK)
    offs_k = jnp.arange(3 * ATTN_BLOCK) - ATTN_BLOCK
    band = jnp.abs(offs_q[:, None] - offs_k[None, :]) <= WINDOW

    def block(i):
        start = i * ATTN_BLOCK
        qb = lax.dynamic_slice_in_dim(q_l, start, ATTN_BLOCK, axis=1)
        kb = lax.dynamic_slice_in_dim(k_pad, start, 3 * ATTN_BLOCK, axis=1)
        vb = lax.dynamic_slice_in_dim(v_pad, start, 3 * ATTN_BLOCK, axis=1)
        kpos = start + offs_k
        valid = band & ((kpos >= 0) & (kpos < L))[None, :]
        s_loc = jnp.einsum("bqkgd,bskd->bkgqs", qb, kb).astype(F32) * scale
        s_loc = jnp.where(valid, s_loc, -1e30)
        s_ctx = jnp.einsum("bqkgd,bckd->bkgqc", qb, k_c).astype(F32) * scale
        p = _sink_softmax(jnp.concatenate([s_loc, s_ctx], axis=-1), sink).astype(vb.dtype)
        return (jnp.einsum("bkgqs,bskd->bqkgd", p[..., :3 * ATTN_BLOCK], vb)
                + jnp.einsum("bkgqc,bckd->bqkgd", p[..., 3 * ATTN_BLOCK:], v_c))

    o_l = lax.map(block, jnp.arange(L // ATTN_BLOCK))
    o_l = jnp.moveaxis(o_l, 0, 1).reshape(B, L, N_HEADS * HEAD_DIM)
    return o_c @ w_o, o_l @ w_o


def _hyena_filter_spectra(L, w1, b1, w2, b2, w3, b3, freq, w4):
    t = jnp.linspace(0.0, 1.0, L, dtype=F32)[:, None]
    omega = (2.0 * math.pi / L) * jnp.arange(L, dtype=F32)[:, None]
    bands = jnp.linspace(1e-4, FILTER_BANDS - 1, FILTER_BANDS, dtype=F32)[None, :]
    z = jnp.concatenate([t, jnp.cos(bands * omega), -jnp.sin(bands * omega)], axis=-1)
    fr = freq.astype(F32)
    hid = jnp.sin(fr * (z @ w1.astype(F32) + b1.astype(F32)))
    hid = jnp.sin(fr * (hid @ w2.astype(F32) + b2.astype(F32)))
    hid = jnp.sin(fr * (hid @ w3.astype(F32) + b3.astype(F32)))
    h = (hid @ w4.astype(F32)).reshape(L, HYENA_ORDER, 2, D_MODEL)
    deltas = jnp.linspace(math.log(DECAY_TARGET) / SLOW_DECAY_PCT,
                          math.log(DECAY_TARGET) / FAST_DECAY_PCT, D_MODEL, dtype=F32)
    h = h * jnp.exp(-t * jnp.abs(deltas))[:, None, None, :]
    k = jnp.concatenate([h[:, :, 0], jnp.zeros((1, HYENA_ORDER, D_MODEL), F32), h[:0:-1, :, 1]], axis=0)
    k = k / jnp.sum(jnp.abs(k), axis=0, keepdims=True)
    return jnp.fft.rfft(k, axis=0)


def _fft_conv(z, k_f, bias):
    L = z.shape[1]
    zf = jnp.fft.rfft(z, n=2 * L, axis=1)
    y = jnp.fft.irfft(zf * k_f[None], n=2 * L, axis=1)[:, :L]
    return y + bias * z


def _hyena_mixer(h_ctx, h_lat, w_in, b_in, conv_w, conv_b, f_w1, f_b1, f_w2, f_b2, f_w3, f_b3,
                 f_freq, f_w4, skip, w_out, b_out):
    def run(h):
        L = h.shape[1]
        u = _dwconv(h @ w_in + b_in, conv_w, conv_b).astype(F32)
        g1, g2, v = jnp.split(u, 3, axis=-1)
        k_f = _hyena_filter_spectra(L, f_w1, f_b1, f_w2, f_b2, f_w3, f_b3, f_freq, f_w4)
        z = g1 * _fft_conv(v, k_f[:, 0], skip[0].astype(F32))
        z = g2 * _fft_conv(z, k_f[:, 1], skip[1].astype(F32))
        return z.astype(h.dtype) @ w_out + b_out
    return run(h_ctx), run(h_lat)


def _swiglu(h, w_gate, w_up, w_down):
    return (jax.nn.silu(h @ w_gate) * (h @ w_up)) @ w_down


def _moe(h, router, w_gate, w_up, w_down):
    logits = (h @ router).astype(F32)
    top_v, top_i = lax.top_k(logits, TOP_K)
    wts = jax.nn.softmax(top_v, axis=-1)
    gate = jnp.sum(jax.nn.one_hot(top_i, N_EXPERTS, dtype=F32) * wts[..., None], axis=-2)
    out = jnp.zeros_like(h)
    for e in range(N_EXPERTS):
        out = out + gate[..., e:e + 1].astype(h.dtype) * _swiglu(h, w_gate[e], w_up[e], w_down[e])
    return out


def setup_inputs(seed: int = 0) -> dict:
    key = jax.random.key(seed)
    keys = iter(jax.random.split(key, 48))
    n_a = len(range(0, DEPTH, N_MIXERS))
    n_b = len(range(1, DEPTH, N_MIXERS))
    n_c = len(range(2, DEPTH, N_MIXERS))
    n_dense = len(range(0, DEPTH, 2))
    n_moe = len(range(1, DEPTH, 2))
    qkv_width = (N_HEADS + 2 * N_KV_HEADS) * HEAD_DIM

    def dense(shape, fan_in, gain=1.0):
        return jax.random.normal(next(keys), shape, F32) * (gain * fan_in ** -0.5)

    def noise(shape, std):
        return jax.random.normal(next(keys), shape, F32) * std

    def norm_gain(shape):
        return 1.0 + noise(shape, 0.05)

    a_init = jax.random.uniform(next(keys), (n_a, 2, D_RNN), F32, 0.9, 0.999) ** (1.0 / LRU_C)
    return {
        "x": noise((BATCH, SEQ, D_MODEL), 1.0),
        "c": noise((BATCH, D_MODEL), 1.0),
        "ctx": noise((BATCH, CTX_LEN, D_MODEL), 1.0),
        "c_ctx": noise((D_MODEL,), 1.0),
        "ada_w": dense((DEPTH, D_MODEL, 6 * D_MODEL), D_MODEL, 0.5),
        "ada_b": noise((DEPTH, 6 * D_MODEL), 0.02),
        "norm_mix": norm_gain((DEPTH, D_MODEL)),
        "norm_ffn": norm_gain((DEPTH, D_MODEL)),
        "norm_final": norm_gain((D_MODEL,)),
        "lru_w_in": dense((n_a, D_MODEL, 2 * D_RNN), D_MODEL),
        "lru_conv_w": dense((n_a, RNN_CONV, D_RNN), RNN_CONV),
        "lru_conv_b": noise((n_a, D_RNN), 0.02),
        "lru_w_a": dense((n_a, 2, RNN_BLOCKS, RNN_BW, RNN_BW), RNN_BW),
        "lru_b_a": noise((n_a, 2, D_RNN), 0.02),
        "lru_w_x": dense((n_a, 2, RNN_BLOCKS, RNN_BW, RNN_BW), RNN_BW),
        "lru_b_x": noise((n_a, 2, D_RNN), 0.02),
        "lru_lambda": jnp.log(a_init) - jnp.log1p(-a_init),
        "lru_w_out": dense((n_a, D_RNN, D_MODEL), D_RNN),
        "attn_w_qkv": dense((n_b, D_MODEL, qkv_width), D_MODEL),
        "attn_sinks": noise((n_b, N_HEADS), 0.5),
        "attn_w_o": dense((n_b, N_HEADS * HEAD_DIM, D_MODEL), N_HEADS * HEAD_DIM),
        "hy_w_in": dense((n_c, D_MODEL, 3 * D_MODEL), D_MODEL),
        "hy_b_in": noise((n_c, 3 * D_MODEL), 0.02),
        "hy_conv_w": dense((n_c, HYENA_CONV, 3 * D_MODEL), HYENA_CONV),
        "hy_conv_b": noise((n_c, 3 * D_MODEL), 0.02),
        "hy_f_w1": dense((n_c, FILTER_EMB, FILTER_WIDTH), FILTER_EMB),
        "hy_f_b1": noise((n_c, FILTER_WIDTH), 0.1),
        "hy_f_w2": dense((n_c, FILTER_WIDTH, FILTER_WIDTH), FILTER_WIDTH),
        "hy_f_b2": noise((n_c, FILTER_WIDTH), 0.1),
        "hy_f_w3": dense((n_c, FILTER_WIDTH, FILTER_WIDTH), FILTER_WIDTH),
        "hy_f_b3": noise((n_c, FILTER_WIDTH), 0.1),
        "hy_f_freq": 1.0 + noise((n_c, FILTER_WIDTH), 0.05),
        "hy_f_w4": dense((n_c, FILTER_WIDTH, HYENA_ORDER * 2 * D_MODEL), FILTER_WIDTH),
        "hy_skip": noise((n_c, HYENA_ORDER, D_MODEL), 1.0),
        "hy_w_out": dense((n_c, D_MODEL, D_MODEL), D_MODEL),
        "hy_b_out": noise((n_c, D_MODEL), 0.02),
        "ffn_w_gate": dense((n_dense, D_MODEL, D_FF), D_MODEL),
        "ffn_w_up": dense((n_dense, D_MODEL, D_FF), D_MODEL),
        "ffn_w_down": dense((n_dense, D_FF, D_MODEL), D_FF),
        "moe_router": dense((n_moe, D_MODEL, N_EXPERTS), D_MODEL),
        "moe_w_gate": dense((n_moe, N_EXPERTS, D_MODEL, D_FF_EXPERT), D_MODEL),
        "moe_w_up": dense((n_moe, N_EXPERTS, D_MODEL, D_FF_EXPERT), D_MODEL),
        "moe_w_down": dense((n_moe, N_EXPERTS, D_FF_EXPERT, D_MODEL), D_FF_EXPERT),
    }


def reference(x, c, ctx, c_ctx, ada_w, ada_b, norm_mix, norm_ffn, norm_final,
              lru_w_in, lru_conv_w, lru_conv_b, lru_w_a, lru_b_a, lru_w_x, lru_b_x, lru_lambda, lru_w_out,
              attn_w_qkv, attn_sinks, attn_w_o,
              hy_w_in, hy_b_in, hy_conv_w, hy_conv_b, hy_f_w1, hy_f_b1, hy_f_w2, hy_f_b2, hy_f_w3, hy_f_b3,
              hy_f_freq, hy_f_w4, hy_skip, hy_w_out, hy_b_out,
              ffn_w_gate, ffn_w_up, ffn_w_down,
              moe_router, moe_w_gate, moe_w_up, moe_w_down):
    rows = x.shape[1] // GRID_W
    rope_cos, rope_sin = _axial_rope_tables(rows)
    s_lat = jax.nn.silu(c)
    s_ctx = jax.nn.silu(c_ctx)
    for i in range(DEPTH):
        j = i // N_MIXERS
        last = i == DEPTH - 1
        mod_l = jnp.split((s_lat @ ada_w[i] + ada_b[i])[:, None, :], 6, axis=-1)
        mod_c = jnp.split(s_ctx @ ada_w[i] + ada_b[i], 6, axis=-1)
        h_c = _ada_norm(ctx, norm_mix[i], mod_c[0], mod_c[1])
        h_l = _ada_norm(x, norm_mix[i], mod_l[0], mod_l[1])
        if i % N_MIXERS == 0:
            y_c, y_l = _rglru_mixer(h_c, h_l, lru_w_in[j], lru_conv_w[j], lru_conv_b[j], lru_w_a[j],
                                    lru_b_a[j], lru_w_x[j], lru_b_x[j], lru_lambda[j], lru_w_out[j])
        elif i % N_MIXERS == 1:
            y_c, y_l = _swa_mixer(h_c, h_l, rope_cos, rope_sin, attn_w_qkv[j], attn_sinks[j], attn_w_o[j])
        else:
            y_c, y_l = _hyena_mixer(h_c, h_l, hy_w_in[j], hy_b_in[j], hy_conv_w[j], hy_conv_b[j],
                                    hy_f_w1[j], hy_f_b1[j], hy_f_w2[j], hy_f_b2[j], hy_f_w3[j], hy_f_b3[j],
                                    hy_f_freq[j], hy_f_w4[j], hy_skip[j], hy_w_out[j], hy_b_out[j])
        x = x + mod_l[2] * y_l

        def channel_mixer(h):
            if i % 2 == 0:
                return _swiglu(h, ffn_w_gate[i // 2], ffn_w_up[i // 2], ffn_w_down[i // 2])
            return _moe(h, moe_router[i // 2], moe_w_gate[i // 2], moe_w_up[i // 2], moe_w_down[i // 2])

        x = x + mod_l[5] * channel_mixer(_ada_norm(x, norm_ffn[i], mod_l[3], mod_l[4]))
        if not last:
            ctx = ctx + mod_c[2] * y_c
            ctx = ctx + mod_c[5] * channel_mixer(_ada_norm(ctx, norm_ffn[i], mod_c[3], mod_c[4]))
    return _rmsnorm(x, norm_final)
```

```python
import math
from contextlib import ExitStack
import numpy as np
import ml_dtypes
import concourse.bass as bass
import concourse.mybir as mybir
from concourse.bass_utils import run_bass_kernel_spmd

F32, BF16, I32 = mybir.dt.float32, mybir.dt.bfloat16, mybir.dt.int32
AF = mybir.ActivationFunctionType
ALU = mybir.AluOpType
AX = mybir.AxisListType

D = 1024
SEQ = 4096
CTX = 256
T = SEQ + CTX
NBLK = T // 128
DFF = 2816
DFE = 3584
NE = 8
EPS = 1e-6
TILES = [(0, 256)] + [(256 + 512 * i, 512) for i in range(8)]


class DSem:
    def __init__(s, sem, name):
        s.sem, s.name, s.cnt = sem, name, 0
        s.twin = None


class Tok:
    __slots__ = ("eng", "val", "dma")

    def __init__(s, eng, val, dma):
        s.eng, s.val, s.dma = eng, val, dma


class Buf:
    def __init__(s, name="", psum=False):
        s.name, s.w, s.r, s.psum = name, None, {}, psum


class Sched:
    NO_RECYCLE = False
    STRICT = True

    def __init__(s, nc, es):
        s.nc, s.es = nc, es
        s.E = dict(pe=nc.tensor, act=nc.scalar, dve=nc.vector, pool=nc.gpsimd, sp=nc.sync)
        s.psem = {e: es.enter_context(nc.semaphore("p_" + e)) for e in s.E}
        s.pcnt = {e: 0 for e in s.E}
        s.pend = {e: False for e in s.E}
        s.seen = {e: {} for e in s.E}
        s.dsems = []
        s.free_ds = []
        s.phase_stack = []
        s.nwait = 0

    def dsem(s, name):
        if s.free_ds and not Sched.NO_RECYCLE:
            d = s.free_ds.pop()
        else:
            d = DSem(s.es.enter_context(s.nc.semaphore("d_" + name)), name)
            s.dsems.append(d)
        if s.phase_stack:
            s.phase_stack[-1].dsems.append(d)
        return d

    def _wait(s, e, sem, key, val):
        if s.seen[e].get(key, 0) >= val:
            return
        s.E[e].wait_ge(sem, val)
        s.seen[e][key] = val
        s.nwait += 1

    def _dep(s, e, t, raw):
        if t.dma is not None:
            s._wait(e, t.dma.sem, "d_" + t.dma.name, t.dma.cnt)
            return
        if t.eng == e:
            if e == "pe":
                return
            if e != "pool" and not Sched.STRICT and not (raw and t.val >= s.pcnt[e]):
                return
        s._wait(e, s.psem[t.eng], "p_" + t.eng, t.val)

    def _deps(s, e, reads, writes):
        for b in reads:
            if b.w is not None:
                s._dep(e, b.w, True)
            if b.psum:
                for t in b.r.values():
                    s._dep(e, t, False)
        for b in writes:
            if b.w is not None:
                s._dep(e, b.w, False)
            for t in b.r.values():
                s._dep(e, t, False)

    def op(s, e, fn, reads=(), writes=(), inc=True):
        s._deps(e, reads, writes)
        inst = fn(s.E[e])
        if inc:
            s.pcnt[e] += 1
            inst.then_inc(s.psem[e], 1)
            t = Tok(e, s.pcnt[e], None)
            s.pend[e] = False
        else:
            t = Tok(e, s.pcnt[e] + 1, None)
            s.pend[e] = True
        for b in reads:
            b.r["p_" + e] = t
        for b in writes:
            b.w = t
            b.r = {}

    def dma(s, q, out, in_, ds, reads=(), writes=(), **kw):
        if q == "pool":
            if ds.twin is None:
                ds.twin = DSem(s.es.enter_context(s.nc.semaphore("w_" + ds.name)), "sw_" + ds.name)
                s.dsems.append(ds.twin)
            ds = ds.twin
        s._deps(q, reads, writes)
        inst = s.E[q].dma_start(out=out, in_=in_, **kw)
        ds.cnt += 16
        inst.then_inc(ds.sem, 16)
        t = Tok(q, ds.cnt, ds)
        for b in reads:
            b.r["d_" + ds.name] = t
        for b in writes:
            b.w = t
            b.r = {}

    def barrier(s):
        for e in s.E:
            assert not s.pend[e], e
        for e in s.E:
            for e2 in s.E:
                if e2 != e and s.pcnt[e2] > 0:
                    s._wait(e, s.psem[e2], "p_" + e2, s.pcnt[e2])
            for d in s.dsems:
                if d.cnt:
                    s._wait(e, d.sem, "d_" + d.name, d.cnt)


class Phase:
    CNT = 0

    def __init__(s, S):
        s.S, s.es = S, ExitStack()
        s.dsems = []
        S.phase_stack.append(s)

    def __enter__(s):
        return s

    def sb(s, shape, dt, name=None):
        Phase.CNT += 1
        t = s.es.enter_context(s.S.nc.sbuf_tensor(f"{name or 't'}_{Phase.CNT}", list(shape), dt))
        return t, Buf(name or "sb")

    def __exit__(s, *a):
        s.S.barrier()
        s.es.close()
        assert s.S.phase_stack.pop() is s
        s.S.free_ds.extend(s.dsems)
        return False


def build(segs=((0, "all"), (1, "all"), (2, "all"), (3, "all")), flags=None):
    flags = flags or {}
    Sched.NO_RECYCLE = bool(flags.get("no_recycle"))
    segs = [(sg, "all") if isinstance(sg, int) else tuple(sg) for sg in segs]
    layers = [L for L, _ in segs]
    parts = {p for _, p in segs}
    need = _needed(segs)
    nc = bass.Bass("TRN2", target_bir_lowering=False)
    es = ExitStack()
    S = Sched(nc, es)

    def din(name, shape, dt=F32):
        if name not in need:
            return None
        return nc.dram_tensor(name, list(shape), dt, kind="ExternalInput").ap()

    def dscr(name, shape, dt=F32):
        return nc.dram_tensor(name, list(shape), dt, kind="Internal").ap()

    xs_in = din("xs_in", [T, D])
    c2T = din("c2T", [128, 8, 2])
    ada_w = din("ada_w", [4, D, 6 * D])
    ada_b = din("ada_b", [4, 6 * D])
    norm_mix = din("norm_mix", [4, D])
    norm_ffn = din("norm_ffn", [4, D])
    norm_final = din("norm_final", [D])
    lru_w_in = din("lru_w_in", [2, D, 2 * D])
    lru_conv_wT = din("lru_conv_wT", [2, 128, 8, 4])
    lru_conv_bT = din("lru_conv_bT", [2, 128, 8])
    lru_w_a = din("lru_w_a", [2, 2, 8, 128, 128])
    lru_b_aT = din("lru_b_aT", [2, 2, 128, 8])
    lru_w_x = din("lru_w_x", [2, 2, 8, 128, 128])
    lru_b_xT = din("lru_b_xT", [2, 2, 128, 8])
    lru_lamT = din("lru_lamT", [2, 2, 128, 8])
    lru_w_out = din("lru_w_out", [2, D, D])
    attn_w_qkv = din("attn_w_qkv", [1, D, 1536])
    attn_w_qkp = din("attn_w_qkp", [1, D, 1280])
    attn_sinks = din("attn_sinks", [1, 16])
    attn_w_o = din("attn_w_o", [1, D, D])
    hy_w_in = din("hy_w_in", [1, D, 3 * D])
    hy_b_inT = din("hy_b_inT", [1, 128, 24])
    hy_conv_wT = din("hy_conv_wT", [1, 128, 24, 3])
    hy_conv_bT = din("hy_conv_bT", [1, 128, 24])
    hy_f_w1 = din("hy_f_w1", [1, 33, 64])
    hy_f_b1 = din("hy_f_b1", [1, 64, 1])
    hy_f_w2 = din("hy_f_w2", [1, 64, 64])
    hy_f_b2 = din("hy_f_b2", [1, 64, 1])
    hy_f_w3 = din("hy_f_w3", [1, 64, 64])
    hy_f_b3 = din("hy_f_b3", [1, 64, 1])
    hy_f_freq = din("hy_f_freq", [1, 64, 1])
    hy_f_w4 = din("hy_f_w4", [1, 64, 4096])
    hy_skip = din("hy_skip", [1, 2, D])
    hy_w_out = din("hy_w_out", [1, D, D])
    hy_b_out = din("hy_b_out", [1, D])
    ffn_w_gate = din("ffn_w_gate", [2, D, DFF])
    ffn_w_up = din("ffn_w_up", [2, D, DFF])
    ffn_w_down = din("ffn_w_down", [2, DFF, D])
    moe_router = din("moe_router", [2, D, NE])
    moe_w_gate = din("moe_w_gate", [2, NE, D, DFE])
    moe_w_up = din("moe_w_up", [2, NE, D, DFE])
    moe_w_down = din("moe_w_down", [2, NE, DFE, D])
    ident_in = din("ident", [128, 128])
    identb_in = din("identb", [128, 128], BF16)
    maskb_in = din("maskb", [128, 384])
    ropec_in = din("ropec", [64, T])
    ropes_in = din("ropes", [64, T])
    ones_in = din("ones", [128, 128])
    absd_in = din("absd", [D])
    zT256_in = din("zT256", [33, 256])
    zT4096_in = din("zT4096", [33, 4096])
    negt256_in = din("negt256", [128, 2])
    negt4096_in = din("negt4096", [128, 32])
    FC256_in = din("FC256", [3, 128, 2, 128], BF16)
    FS256_in = din("FS256", [3, 128, 2, 128], BF16)
    GC256_in = din("GC256", [2, 128, 3, 128], BF16)
    GS256_in = din("GS256", [2, 128, 3, 128], BF16)
    FC4096_in = din("FC4096", [33, 128, 32, 128], BF16)
    FS4096_in = din("FS4096", [33, 128, 32, 128], BF16)
    GC4096_in = din("GC4096", [32, 128, 33, 128], BF16)
    GS4096_in = din("GS4096", [32, 128, 33, 128], BF16)
    out = nc.dram_tensor("out", [SEQ, D], F32, kind="ExternalOutput").ap()
    xs_out = nc.dram_tensor("xs_out", [T, D], F32, kind="ExternalOutput").ap()

    XS = dscr("XS", [T, D])
    MODS = dscr("MODS", [4, 2, 6 * D])
    if "mix" in parts:
        H2T = nc.dram_tensor("h2t_out", [8, 128, T], BF16, kind="ExternalOutput").ap()
        GATES = nc.dram_tensor("gts_out", [128, NBLK * NE], F32, kind="ExternalOutput").ap()
    elif "ffn" in parts:
        H2T = din("h2t_in", [8, 128, T], BF16)
        GATES = din("gts_in", [128, NBLK * NE])
    else:
        H2T = dscr("H2T", [8, 128, T], BF16)
        GATES = dscr("GATES", [128, NBLK * NE])
    gdB = Buf("gatesdram")
    OT = dscr("OT", [8, 128, T], BF16)
    GEL = dscr("GEL", [8, 128, T], BF16)
    UPRE = dscr("UPRE", [8, 128, T])
    xsbuf = [Buf(f"xs{i}") for i in range(NBLK)]
    otbuf = [Buf(f"ot{i}") for i in range(len(TILES))]
    h2buf = [Buf(f"h2{i}") for i in range(len(TILES))]

    ps = []
    for i in range(8):
        t = es.enter_context(nc.psum_tensor(f"ps{i}", [128, 512], F32))
        ps.append((t, Buf(f"ps{i}", psum=True)))
    psi = [0]

    def nps():
        psi[0] = (psi[0] + 1) % 8
        return ps[psi[0]]

    G = Phase(S)
    ident, identB = G.sb([128, 128], F32, "ident")
    identb, identbB = G.sb([128, 128], BF16, "identb")
    cds = S.dsem("const")
    S.dma("sp", ident[:], ident_in, cds, writes=[identB])
    S.dma("sp", identb[:], identb_in, cds, writes=[identbB])
    gates, gatesB = G.sb([128, NBLK, NE], F32, "gates")

    with Phase(S) as P:
        xcp = S.dsem("xcp")
        S.dma("sp", XS[:, :], xs_in, xcp, writes=xsbuf)
        c2, c2B = P.sb([128, 8, 2], F32, "c2")
        s2, s2B = P.sb([128, 8, 2], F32, "s2")
        S.dma("sp", c2[:], c2T, cds, writes=[c2B])
        S.op("act", lambda e: e.activation(out=s2[:], in_=c2[:], func=AF.Silu), reads=[c2B], writes=[s2B])
        wsl = [P.sb([128, 8, 512], F32, f"adaw{i}") + (S.dsem(f"adaw{i}"),) for i in range(2)]
        mrow, mrowB = P.sb([2, 6 * D], F32, "mrow")
        brow, browB = P.sb([2, 6 * D], F32, "brow")
        grow, growB = P.sb([2, 2 * D], F32, "grow")
        mds = S.dsem("mods")
        k = 0
        for L in layers:
            S.dma("sp", brow[:], ada_b[L, :].partition_broadcast(2), mds, writes=[browB])
            S.dma("sp", grow[:, 0:D], norm_mix[L, :].partition_broadcast(2), mds, writes=[growB])
            S.dma("sp", grow[:, D:2 * D], norm_ffn[L, :].partition_broadcast(2), mds, writes=[growB])
            for cg in range(12):
                w, wB, wd = wsl[k % 2]
                k += 1
                S.dma("sp", w[:], ada_w[L, :, cg * 512:(cg + 1) * 512].rearrange("(kc p) n -> p kc n", p=128), wd, writes=[wB])
                pt, pB = nps()
                for kc in range(8):
                    S.op("pe", lambda e, kc=kc, pt=pt, w=w: e.matmul(pt[0:2, :], s2[:, kc, :], w[:, kc, :], start=(kc == 0), stop=(kc == 7)),
                         reads=[s2B, wB], writes=[pB], inc=(kc == 7))
                S.op("dve", lambda e, pt=pt, cg=cg: e.tensor_tensor(out=mrow[:, cg * 512:(cg + 1) * 512], in0=pt[0:2, :], in1=brow[:, cg * 512:(cg + 1) * 512], op=ALU.add),
                     reads=[pB, browB], writes=[mrowB])
            for ch, go in ((1, 0), (4, D)):
                S.op("dve", lambda e, ch=ch, go=go: e.scalar_tensor_tensor(out=mrow[:, ch * D:(ch + 1) * D], in0=mrow[:, ch * D:(ch + 1) * D], scalar=1.0,
                                                                         in1=grow[:, go:go + D], op0=ALU.add, op1=ALU.mult),
                     reads=[mrowB, growB], writes=[mrowB])
            S.dma("sp", MODS[L, :, :], mrow[:], mds, reads=[mrowB])

    def load_mods(P, L, which, ks, ds):
        res = {}
        for kk in ks:
            t, tB = P.sb([128, D], F32, f"mod{L}_{which}_{kk}")
            S.dma("sp", t[:], MODS[L, which, kk * D:(kk + 1) * D].partition_broadcast(128), ds, writes=[tB])
            res[kk] = (t, tB)
        return res

    def rms_rstd(P, xt, xB, junk, junkB, st):
        stt, stB = st
        S.op("act", lambda e: e.activation(out=junk[:], in_=xt[:], func=AF.Square, accum_out=stt[:, 0:1]), reads=[xB], writes=[junkB, stB])
        S.op("act", lambda e: e.activation(out=stt[:, 1:2], in_=stt[:, 0:1], func=AF.Sqrt, scale=1.0 / D, bias=EPS), reads=[stB], writes=[stB])
        S.op("dve", lambda e: e.reciprocal(out=stt[:, 2:3], in_=stt[:, 1:2]), reads=[stB], writes=[stB])

    def norm_to_hT(xt, xB, tmp, tmpB, junk, junkB, st, A, sh, hT, hTB, col0, h32=None):
        rms_rstd(None, xt, xB, junk, junkB, st)
        stt, stB = st
        S.op("dve", lambda e: e.scalar_tensor_tensor(out=tmp[:], in0=xt[:], scalar=stt[:, 2:3], in1=A[0][:], op0=ALU.mult, op1=ALU.mult),
             reads=[xB, stB, A[1]], writes=[tmpB])
        S.op("dve", lambda e: e.tensor_tensor(out=tmp[:], in0=tmp[:], in1=sh[0][:], op=ALU.add), reads=[tmpB, sh[1]], writes=[tmpB])
        for half in range(2):
            pt, pB = nps()
            for q in range(4):
                kc = half * 4 + q
                S.op("pe", lambda e, pt=pt, q=q, kc=kc: e.transpose(pt[:, q * 128:(q + 1) * 128], tmp[:, kc * 128:(kc + 1) * 128], ident[:]),
                     reads=[tmpB, identB], writes=[pB], inc=(q == 3))
            S.op("act", lambda e, pt=pt, half=half: e.copy(out=hT[:, half * 4:half * 4 + 4, col0:col0 + 128], in_=pt[:, :].rearrange("p (a b) -> p a b", a=4)),
                 reads=[pB], writes=[hTB])
            if h32 is not None:
                S.op("dve", lambda e, pt=pt, half=half: e.tensor_copy(out=h32[0][:, half * 4:half * 4 + 4, :], in_=pt[:, :].rearrange("p (a b) -> p a b", a=4)),
                     reads=[pB], writes=[h32[1], pB])

    def phase_norm_proj(L, proj_setup, proj_tile, last):
        with Phase(S) as P:
            mds = S.dsem(f"m1_{L}")
            modl = load_mods(P, L, 0, (0, 1), mds)
            modc = load_mods(P, L, 1, (0, 1), mds)
            ctxo = proj_setup(P)
            xsl = [P.sb([128, D], F32, f"x{i}") + (S.dsem(f"x1_{L}_{i}"),) for i in range(3)]
            tmp, tmpB = P.sb([128, D], F32, "tmp")
            junk, junkB = P.sb([128, D], BF16, "junk")
            st = P.sb([128, 4], F32, "st")
            hTs = [P.sb([128, 8, 512], BF16, f"hT{i}") for i in range(2)]
            xi = 0
            for ti, (t0, ntok) in enumerate(TILES):
                hT, hTB = hTs[ti % 2]
                md = modc if ti == 0 else modl
                for b in range(ntok // 128):
                    blk = t0 // 128 + b
                    xt, xB, xd = xsl[xi % 3]
                    xi += 1
                    S.dma("sp", xt[:], XS[blk * 128:(blk + 1) * 128, :], xd, reads=[xsbuf[blk]], writes=[xB])
                    norm_to_hT(xt, xB, tmp, tmpB, junk, junkB, st, md[1], md[0], hT, hTB, b * 128)
                proj_tile(P, ctxo, ti, t0, ntok, hT, hTB)

    def phase_outproj_norm2(L, w_out_ap, bias_ap, last, moe_idx):
        if flags.get("no_moe_all"):
            moe_idx = None
        with Phase(S) as P:
            mds = S.dsem(f"m3_{L}")
            modl = load_mods(P, L, 0, (2, 3, 4), mds)
            modc = load_mods(P, L, 1, (2, 3, 4), mds)
            wo, woB = P.sb([128, 8, D], BF16, "wo")
            S.dma("pool", wo[:], w_out_ap.rearrange("(kc p) n -> p kc n", p=128), mds, writes=[woB])
            bo = None
            if bias_ap is not None:
                bo = P.sb([128, D], F32, "bo")
                S.dma("sp", bo[0][:], bias_ap.partition_broadcast(128), mds, writes=[bo[1]])
            if moe_idx is not None:
                rt, rtB = P.sb([128, 8, NE], F32, "rt")
                S.dma("sp", rt[:], moe_router[moe_idx].rearrange("(kc p) n -> p kc n", p=128), mds, writes=[rtB])
                h32 = P.sb([128, 8, 128], F32, "h32")
                lg, lgB = P.sb([128, NE], F32, "lg")
                m8, m8B = P.sb([128, 8], F32, "m8")
                wv, wvB = P.sb([128, 4], F32, "wv")
                e1, e1B = P.sb([128, NE], F32, "e1")
            ots = [P.sb([128, 8, 512], BF16, f"ot{i}") + (S.dsem(f"ot3_{L}_{i}"),) for i in range(2)]
            xsl = [P.sb([128, D], F32, f"x{i}") + (S.dsem(f"x3_{L}_{i}"),) for i in range(3)]
            tmp, tmpB = P.sb([128, D], F32, "tmp")
            tmp2, tmp2B = P.sb([128, D], F32, "tmp2")
            junk, junkB = P.sb([128, D], BF16, "junk")
            st = P.sb([128, 4], F32, "st")
            hTs = [P.sb([128, 8, 512], BF16, f"hT{i}") + (S.dsem(f"h2s_{L}_{i}"),) for i in range(2)]
            xi = 0
            for ti, (t0, ntok) in enumerate(TILES):
                if last and ti == 0:
                    continue
                ot, otB, otd = ots[ti % 2]
                S.dma("sp", ot[:, :, 0:ntok], OT[:, :, t0:t0 + ntok].rearrange("j p t -> p j t"), otd, reads=[otbuf[ti]], writes=[otB])
                hT, hTB, hTd = hTs[ti % 2]
                md = modc if ti == 0 else modl
                for b in range(ntok // 128):
                    blk = t0 // 128 + b
                    xt, xB, xd = xsl[xi % 3]
                    xi += 1
                    S.dma("sp", xt[:], XS[blk * 128:(blk + 1) * 128, :], xd, reads=[xsbuf[blk]], writes=[xB])
                    for half in range(2):
                        pt, pB = nps()
                        for kc in range(8):
                            S.op("pe", lambda e, pt=pt, kc=kc, half=half, ot=ot, b=b: e.matmul(pt[:, :], ot[:, kc, b * 128:(b + 1) * 128], wo[:, kc, half * 512:(half + 1) * 512],
                                                                                          start=(kc == 0), stop=(kc == 7)),
                                 reads=[otB, woB], writes=[pB], inc=(kc == 7))
                        hs = slice(half * 512, (half + 1) * 512)
                        if bo is not None:
                            S.op("dve", lambda e, pt=pt, hs=hs: e.tensor_tensor(out=tmp2[:, hs], in0=pt[:, :], in1=bo[0][:, hs], op=ALU.add), reads=[pB, bo[1]], writes=[tmp2B])
                            S.op("dve", lambda e, hs=hs, md=md: e.tensor_tensor(out=tmp2[:, hs], in0=tmp2[:, hs], in1=md[2][0][:, hs], op=ALU.mult), reads=[tmp2B, md[2][1]], writes=[tmp2B])
                        else:
                            S.op("dve", lambda e, pt=pt, hs=hs, md=md: e.tensor_tensor(out=tmp2[:, hs], in0=pt[:, :], in1=md[2][0][:, hs], op=ALU.mult), reads=[pB, md[2][1]], writes=[tmp2B])
                    S.op("dve", lambda e, xt=xt: e.tensor_tensor(out=xt[:], in0=xt[:], in1=tmp2[:], op=ALU.add), reads=[xB, tmp2B], writes=[xB])
                    S.dma("sp", XS[blk * 128:(blk + 1) * 128, :], xt[:], xd, reads=[xB], writes=[xsbuf[blk]])
                    norm_to_hT(xt, xB, tmp, tmpB, junk, junkB, st, md[4], md[3], hT, hTB, b * 128, h32=(h32 if moe_idx is not None else None))
                    if moe_idx is not None and not flags.get("no_gate"):
                        pt, pB = nps()
                        for kc in range(8):
                            S.op("pe", lambda e, pt=pt, kc=kc: e.matmul(pt[:, 0:NE], h32[0][:, kc, :], rt[:, kc, :], start=(kc == 0), stop=(kc == 7)),
                                 reads=[h32[1], rtB], writes=[pB], inc=(kc == 7))
                        S.op("act", lambda e, pt=pt: e.copy(out=lg[:], in_=pt[:, 0:NE]), reads=[pB], writes=[lgB])
                        S.op("dve", lambda e: e.max(out=m8[:], in_=lg[:]), reads=[lgB], writes=[m8B])
                        S.op("dve", lambda e: e.tensor_tensor(out=wv[:, 0:1], in0=m8[:, 0:1], in1=m8[:, 1:2], op=ALU.subtract), reads=[m8B], writes=[wvB])
                        S.op("act", lambda e: e.activation(out=wv[:, 1:2], in_=wv[:, 0:1], func=AF.Sigmoid), reads=[wvB], writes=[wvB])
                        S.op("act", lambda e: e.activation(out=wv[:, 2:3], in_=wv[:, 0:1], func=AF.Sigmoid, scale=-1.0), reads=[wvB], writes=[wvB])
                        S.op("dve", lambda e: e.tensor_scalar(out=e1[:], in0=lg[:], scalar1=m8[:, 0:1], scalar2=wv[:, 1:2], op0=ALU.is_equal, op1=ALU.mult),
                             reads=[lgB, m8B, wvB], writes=[e1B])
                        S.op("dve", lambda e: e.tensor_scalar(out=lg[:], in0=lg[:], scalar1=m8[:, 1:2], scalar2=wv[:, 2:3], op0=ALU.is_equal, op1=ALU.mult),
                             reads=[lgB, m8B, wvB], writes=[lgB])
                        S.op("dve", lambda e, blk=blk: e.tensor_tensor(out=gates[:, blk, :], in0=e1[:], in1=lg[:], op=ALU.add), reads=[e1B, lgB], writes=[gatesB])
                S.dma("sp", H2T[:, :, t0:t0 + ntok].rearrange("j p t -> p j t"), hT[:, :, 0:ntok], hTd, reads=[hTB], writes=[h2buf[ti]])
            if moe_idx is not None and not flags.get("no_gstore"):
                S.dma("sp", GATES, gates[:].rearrange("p a b -> p (a b)"), mds, reads=[gatesB], writes=[gdB])

    def phase_ffn(L, dense_idx, moe_idx, last):
        if moe_idx is None:
            experts = [(ffn_w_gate[dense_idx], ffn_w_up[dense_idx], ffn_w_down[dense_idx])]
            dff = DFF
        else:
            experts = [(moe_w_gate[moe_idx, e], moe_w_up[moe_idx, e], moe_w_down[moe_idx, e]) for e in range(flags.get('moe_ne', NE))]
            dff = DFE
        groups = [(f0, min(512, dff - f0)) for f0 in range(0, dff, 512)]
        if last:
            supers = [[1, 2, 3, 4], [5, 6, 7, 8]]
        else:
            supers = [[0, 1, 2, 3, 4], [5, 6, 7, 8]]
        with Phase(S) as P:
            mds = S.dsem(f"m4_{L}")
            gl = load_mods(P, L, 0, (5,), mds)[5]
            if moe_idx is not None:
                S.dma("sp", gates[:].rearrange("p a b -> p (a b)"), GATES, mds, reads=[gdB], writes=[gatesB])
            gc = load_mods(P, L, 1, (5,), mds)[5]
            hT, hTB = P.sb([128, 8, 2304], BF16, "hTs")
            hd = S.dsem(f"h4_{L}")
            acc, accB = P.sb([128, 18, D], F32, "acc")
            wsl = []
            for i in range(2):
                wg, wgB = P.sb([128, 8, 512], BF16, f"wg{i}")
                wu, wuB = P.sb([128, 8, 512], BF16, f"wu{i}")
                wd, wdB = P.sb([128, 4, D], BF16, f"wd{i}")
                wsl.append((wg, wgB, wu, wuB, wd, wdB, S.dsem(f"w4_{L}_{i}")))
            acts = [P.sb([128, 4, 512], BF16, f"act{i}") for i in range(2)]
            sg = [P.sb([128, 512], F32, f"sg{i}") for i in range(2)]
            xsl = [P.sb([128, D], F32, f"x{i}") + (S.dsem(f"x4_{L}_{i}"),) for i in range(2)]
            tmp, tmpB = P.sb([128, D], F32, "tmp")
            wi = 0
            ai = 0
            xi = 0
            for sup in supers:
                col = 0
                cols = []
                for ti in sup:
                    t0, ntok = TILES[ti]
                    S.dma("sp", hT[:, :, col:col + ntok], H2T[:, :, t0:t0 + ntok].rearrange("j p t -> p j t"), hd, reads=[h2buf[ti]], writes=[hTB])
                    cols.append((col, ntok, t0))
                    col += ntok
                nblk = col // 128
                first = True
                for ei, (wg_ap, wu_ap, wd_ap) in enumerate(experts):
                    for gi, (f0, fw) in enumerate(groups):
                        nfc = fw // 128
                        wg, wgB, wu, wuB, wd, wdB, wds = wsl[wi % 2]
                        wi += 1
                        S.dma("pool", wg[:, :, 0:fw], wg_ap[:, f0:f0 + fw].rearrange("(kc p) n -> p kc n", p=128), wds, writes=[wgB])
                        S.dma("pool", wu[:, :, 0:fw], wu_ap[:, f0:f0 + fw].rearrange("(kc p) n -> p kc n", p=128), wds, writes=[wuB])
                        S.dma("pool", wd[:, 0:nfc, :], wd_ap[f0:f0 + fw, :].rearrange("(fc p) n -> p fc n", p=128), wds, writes=[wdB])
                        for (c0, ntok, t0) in cols:
                            act, actB = acts[ai % 2]
                            ai += 1
                            for fc in range(nfc):
                                pg, pgB = nps()
                                pu, puB = nps()
                                for kc in range(8):
                                    S.op("pe", lambda e, pg=pg, kc=kc, fc=fc, wg=wg, c0=c0, ntok=ntok: e.matmul(pg[:, 0:ntok], wg[:, kc, fc * 128:(fc + 1) * 128], hT[:, kc, c0:c0 + ntok],
                                                                                                             start=(kc == 0), stop=(kc == 7)),
                                         reads=[wgB, hTB], writes=[pgB], inc=(kc == 7))
                                for kc in range(8):
                                    S.op("pe", lambda e, pu=pu, kc=kc, fc=fc, wu=wu, c0=c0, ntok=ntok: e.matmul(pu[:, 0:ntok], wu[:, kc, fc * 128:(fc + 1) * 128], hT[:, kc, c0:c0 + ntok],
                                                                                                             start=(kc == 0), stop=(kc == 7)),
                                         reads=[wuB, hTB], writes=[puB], inc=(kc == 7))
                                sgt, sgB = sg[fc % 2]
                                S.op("act", lambda e, pg=pg, sgt=sgt, ntok=ntok: e.activation(out=sgt[:, 0:ntok], in_=pg[:, 0:ntok], func=AF.Silu), reads=[pgB], writes=[sgB])
                                S.op("dve", lambda e, pu=pu, sgt=sgt, act=act, fc=fc, ntok=ntok: e.tensor_tensor(out=act[:, fc, 0:ntok], in0=pu[:, 0:ntok], in1=sgt[:, 0:ntok], op=ALU.mult),
                                     reads=[puB, sgB], writes=[actB])
                            for b in range(ntok // 128):
                                ab = (c0 // 128) + b
                                blk = t0 // 128 + b
                                for half in range(2):
                                    pd, pdB = nps()
                                    for fc in range(nfc):
                                        S.op("pe", lambda e, pd=pd, fc=fc, act=act, b=b, wd=wd, half=half: e.matmul(pd[:, :], act[:, fc, b * 128:(b + 1) * 128], wd[:, fc, half * 512:(half + 1) * 512],
                                                                                                                 start=(fc == 0), stop=(fc == nfc - 1)),
                                             reads=[actB, wdB], writes=[pdB], inc=(fc == nfc - 1))
                                    hs = slice(half * 512, (half + 1) * 512)
                                    if moe_idx is None:
                                        if first:
                                            S.op("dve", lambda e, pd=pd, ab=ab, hs=hs: e.tensor_copy(out=acc[:, ab, hs], in_=pd[:, :]), reads=[pdB], writes=[accB])
                                        else:
                                            S.op("dve", lambda e, pd=pd, ab=ab, hs=hs: e.tensor_tensor(out=acc[:, ab, hs], in0=pd[:, :], in1=acc[:, ab, hs], op=ALU.add), reads=[pdB, accB], writes=[accB])
                                    else:
                                        if first:
                                            S.op("dve", lambda e, pd=pd, ab=ab, hs=hs, blk=blk, ei=ei: e.tensor_scalar(out=acc[:, ab, hs], in0=pd[:, :], scalar1=gates[:, blk, ei:ei + 1], scalar2=None, op0=ALU.mult),
                                                 reads=[pdB, gatesB], writes=[accB])
                                        else:
                                            S.op("dve", lambda e, pd=pd, ab=ab, hs=hs, blk=blk, ei=ei: e.scalar_tensor_tensor(out=acc[:, ab, hs], in0=pd[:, :], scalar=gates[:, blk, ei:ei + 1], in1=acc[:, ab, hs],
                                                                                                                         op0=ALU.mult, op1=ALU.add),
                                                 reads=[pdB, gatesB, accB], writes=[accB])
                        first = False
                for (c0, ntok, t0) in cols:
                    gm = gc if t0 == 0 else gl
                    for b in range(ntok // 128):
                        ab = (c0 // 128) + b
                        blk = t0 // 128 + b
                        xt, xB, xd = xsl[xi % 2]
                        xi += 1
                        S.dma("sp", xt[:], XS[blk * 128:(blk + 1) * 128, :], xd, reads=[xsbuf[blk]], writes=[xB])
                        S.op("dve", lambda e, ab=ab, gm=gm: e.tensor_tensor(out=tmp[:], in0=acc[:, ab, :], in1=gm[0][:], op=ALU.mult), reads=[accB, gm[1]], writes=[tmpB])
                        S.op("dve", lambda e, xt=xt: e.tensor_tensor(out=xt[:], in0=xt[:], in1=tmp[:], op=ALU.add), reads=[xB, tmpB], writes=[xB])
                        S.dma("sp", XS[blk * 128:(blk + 1) * 128, :], xt[:], xd, reads=[xB], writes=[xsbuf[blk]])

    def mixer_rglru(L, j, last):
        def setup(P):
            w, wB = P.sb([128, 8, 2 * D], BF16, "lruwin")
            d = S.dsem(f"lw_{L}")
            for h in range(2):
                S.dma("pool", w[:, :, h * D:(h + 1) * D], lru_w_in[j, :, h * D:(h + 1) * D].rearrange("(kc p) n -> p kc n", p=128), d, writes=[wB])
            gs = [P.sb([128, 8, 512], BF16, f"gst{i}") + (S.dsem(f"gst_{L}_{i}"),) for i in range(2)]
            us = [P.sb([128, 8, 512], F32, f"ust{i}") + (S.dsem(f"ust_{L}_{i}"),) for i in range(2)]
            return dict(w=w, wB=wB, gs=gs, us=us)

        def tile(P, c, ti, t0, ntok, hT, hTB):
            g, gB, gd = c["gs"][ti % 2]
            u, uB, ud = c["us"][ti % 2]
            w, wB = c["w"], c["wB"]
            for jj in range(8):
                pg, pgB = nps()
                for kc in range(8):
                    S.op("pe", lambda e, pg=pg, kc=kc, jj=jj: e.matmul(pg[:, 0:ntok], w[:, kc, jj * 128:(jj + 1) * 128], hT[:, kc, 0:ntok], start=(kc == 0), stop=(kc == 7)),
                         reads=[wB, hTB], writes=[pgB], inc=(kc == 7))
                S.op("act", lambda e, pg=pg, jj=jj: e.activation(out=g[:, jj, 0:ntok], in_=pg[:, 0:ntok], func=AF.Gelu_apprx_tanh), reads=[pgB], writes=[gB])
                pu, puB = nps()
                for kc in range(8):
                    S.op("pe", lambda e, pu=pu, kc=kc, jj=jj: e.matmul(pu[:, 0:ntok], w[:, kc, D + jj * 128:D + (jj + 1) * 128], hT[:, kc, 0:ntok], start=(kc == 0), stop=(kc == 7)),
                         reads=[wB, hTB], writes=[puB], inc=(kc == 7))
                S.op("dve", lambda e, pu=pu, jj=jj: e.tensor_copy(out=u[:, jj, 0:ntok], in_=pu[:, 0:ntok]), reads=[puB], writes=[uB])
            S.dma("sp", GEL[:, :, t0:t0 + ntok].rearrange("j p t -> p j t"), g[:, :, 0:ntok], gd, reads=[gB])
            S.dma("sp", UPRE[:, :, t0:t0 + ntok].rearrange("j p t -> p j t"), u[:, :, 0:ntok], ud, reads=[uB])

        phase_norm_proj(L, setup, tile, last)

        with Phase(S) as P:
            cd = S.dsem(f"lc_{L}")
            cw, cwB = P.sb([128, 8, 4], F32, "cw")
            cb, cbB = P.sb([128, 8], F32, "cb")
            ba, baB = P.sb([128, 2, 8], F32, "ba")
            bx, bxB = P.sb([128, 2, 8], F32, "bx")
            lam, lamB = P.sb([128, 2, 8], F32, "lam")
            cdv, cdvB = P.sb([128, 2, 8], F32, "cdv")
            cdv2, cdv2B = P.sb([128, 2, 8], F32, "cdv2")
            S.dma("sp", cw[:], lru_conv_wT[j], cd, writes=[cwB])
            S.dma("sp", cb[:], lru_conv_bT[j], cd, writes=[cbB])
            for d in range(2):
                S.dma("sp", ba[:, d, :], lru_b_aT[j, d], cd, writes=[baB])
                S.dma("sp", bx[:, d, :], lru_b_xT[j, d], cd, writes=[bxB])
                S.dma("sp", lam[:, d, :], lru_lamT[j, d], cd, writes=[lamB])
            S.op("act", lambda e: e.activation(out=cdv[:], in_=lam[:], func=AF.Exp, scale=-1.0), reads=[lamB], writes=[cdvB])
            S.op("act", lambda e: e.activation(out=cdv[:], in_=cdv[:], func=AF.Ln, bias=1.0), reads=[cdvB], writes=[cdvB])
            S.op("dve", lambda e: e.tensor_scalar(out=cdv2[:], in0=cdv[:], scalar1=-16.0, scalar2=None, op0=ALU.mult), reads=[cdvB], writes=[cdv2B])
            S.op("dve", lambda e: e.tensor_scalar(out=cdv[:], in0=cdv[:], scalar1=-8.0, scalar2=None, op0=ALU.mult), reads=[cdvB], writes=[cdvB])
            wa, waB = P.sb([128, 2, 8, 128], BF16, "wa")
            wx, wxB = P.sb([128, 2, 8, 128], BF16, "wx")
            for d in range(2):
                S.dma("pool", wa[:, d, :, :], lru_w_a[j, d].rearrange("h i o -> i h o"), cd, writes=[waB])
                S.dma("pool", wx[:, d, :, :], lru_w_x[j, d].rearrange("h i o -> i h o"), cd, writes=[wxB])
            UP, UPB = P.sb([128, T], F32, "UP")
            U, UB_ = P.sb([128, T], F32, "U")
            Ub, UbB = P.sb([128, T], BF16, "Ub")
            Rr, RB = P.sb([128, T], F32, "R")
            Ii, IB = P.sb([128, T], F32, "I")
            Aa, AB = P.sb([128, T], F32, "A")
            Bb, BB = P.sb([128, T], F32, "Bv")
            Y0, Y0B = P.sb([128, T], F32, "Y0")
            Y1, Y1B = P.sb([128, T], F32, "Y1")
            Gg, GB = P.sb([128, T], BF16, "Gg")
            Oo, OB = P.sb([128, T], BF16, "Oo")
            ld = S.dsem(f"ll_{L}")
            ld2 = S.dsem(f"ll2_{L}")
            segs = [(0, CTX), (CTX, T)]
            for jj in range(8):
                S.dma("sp", UP[:], UPRE[jj, :, :], ld, writes=[UPB])
                S.dma("sp", Gg[:], GEL[jj, :, :], ld2, writes=[GB])
                S.op("act", lambda e, jj=jj: e.activation(out=U[:], in_=UP[:], func=AF.Identity, scale=cw[:, jj, 2:3], bias=cb[:, jj:jj + 1]), reads=[UPB, cwB, cbB], writes=[UB_])
                for (a, b_) in segs:
                    for (tap, sh) in ((0, -2), (1, -1), (3, 1)):
                        lo = max(a, a - sh)
                        hi = min(b_, b_ - sh)
                        S.op("dve", lambda e, jj=jj, tap=tap, sh=sh, lo=lo, hi=hi: e.scalar_tensor_tensor(out=U[:, lo:hi], in0=UP[:, lo + sh:hi + sh], scalar=cw[:, jj, tap:tap + 1], in1=U[:, lo:hi],
                                                                                                   op0=ALU.mult, op1=ALU.add),
                             reads=[UPB, cwB, UB_], writes=[UB_])
                S.op("act", lambda e: e.copy(out=Ub[:], in_=U[:]), reads=[UB_], writes=[UbB])
                for d in range(2):
                    for (t0, ntok) in TILES:
                        pr, prB = nps()
                        S.op("pe", lambda e, pr=pr, d=d, jj=jj, t0=t0, ntok=ntok: e.matmul(pr[:, 0:ntok], wa[:, d, jj, :], Ub[:, t0:t0 + ntok], start=True, stop=True), reads=[waB, UbB], writes=[prB])
                        S.op("act", lambda e, pr=pr, d=d, jj=jj, t0=t0, ntok=ntok: e.activation(out=Rr[:, t0:t0 + ntok], in_=pr[:, 0:ntok], func=AF.Sigmoid, bias=ba[:, d, jj:jj + 1]), reads=[prB, baB], writes=[RB])
                        pi_, piB = nps()
                        S.op("pe", lambda e, pi_=pi_, d=d, jj=jj, t0=t0, ntok=ntok: e.matmul(pi_[:, 0:ntok], wx[:, d, jj, :], Ub[:, t0:t0 + ntok], start=True, stop=True), reads=[wxB, UbB], writes=[piB])
                        S.op("act", lambda e, pi_=pi_, d=d, jj=jj, t0=t0, ntok=ntok: e.activation(out=Ii[:, t0:t0 + ntok], in_=pi_[:, 0:ntok], func=AF.Sigmoid, bias=bx[:, d, jj:jj + 1]), reads=[piB, bxB], writes=[IB])
                    S.op("act", lambda e, d=d, jj=jj: e.activation(out=Aa[:], in_=Rr[:], func=AF.Exp, scale=cdv[:, d, jj:jj + 1]), reads=[RB, cdvB], writes=[AB])
                    S.op("act", lambda e, d=d, jj=jj: e.activation(out=Rr[:], in_=Rr[:], func=AF.Exp, scale=cdv2[:, d, jj:jj + 1]), reads=[RB, cdv2B], writes=[RB])
                    S.op("dve", lambda e: e.tensor_scalar(out=Rr[:], in0=Rr[:], scalar1=-1.0, scalar2=1.0, op0=ALU.mult, op1=ALU.add), reads=[RB], writes=[RB])
                    S.op("act", lambda e: e.activation(out=Rr[:], in_=Rr[:], func=AF.Sqrt), reads=[RB], writes=[RB])
                    S.op("dve", lambda e: e.tensor_tensor(out=Ii[:], in0=Ii[:], in1=U[:], op=ALU.mult), reads=[IB, UB_], writes=[IB])
                    S.op("dve", lambda e: e.tensor_tensor(out=Bb[:], in0=Rr[:], in1=Ii[:], op=ALU.mult), reads=[RB, IB], writes=[BB])
                    if d == 0:
                        S.op("dve", lambda e: e.tensor_tensor_scan(out=Y0[:], data0=Aa[:], data1=Bb[:], initial=0.0, op0=ALU.mult, op1=ALU.add), reads=[AB, BB], writes=[Y0B])
                    else:
                        S.op("dve", lambda e: e.tensor_tensor_scan(out=Y1[:, CTX - 1::-1], data0=Aa[:, CTX - 1::-1], data1=Bb[:, CTX - 1::-1], initial=0.0, op0=ALU.mult, op1=ALU.add),
                             reads=[AB, BB], writes=[Y1B])
                        S.op("dve", lambda e: e.tensor_tensor_scan(out=Y1[:, T - 1:CTX - 1:-1], data0=Aa[:, T - 1:CTX - 1:-1], data1=Bb[:, T - 1:CTX - 1:-1], initial=Y1[:, 0:1], op0=ALU.mult, op1=ALU.add),
                             reads=[AB, BB, Y1B], writes=[Y1B])
                S.op("dve", lambda e: e.tensor_tensor(out=Y0[:], in0=Y0[:], in1=Y1[:], op=ALU.add), reads=[Y0B, Y1B], writes=[Y0B])
                S.op("dve", lambda e: e.tensor_tensor(out=Oo[:], in0=Y0[:], in1=Gg[:], op=ALU.mult), reads=[Y0B, GB], writes=[OB])
                S.dma("sp", OT[jj, :, :], Oo[:], ld2, reads=[OB], writes=otbuf)
        phase_outproj_norm2(L, lru_w_out[j], None, last, (L // 2) if L % 2 == 1 else None)


    def mixer_swa(L, last):
        QT = dscr("QT", [16, 64, T], BF16)
        KT = dscr("KT", [4, 64, T], BF16)
        VV = dscr("VV", [T, 256], BF16)
        SC = 0.125

        def setup(P):
            d = S.dsem(f"aw_{L}")
            w, wB = P.sb([128, 8, 1536], BF16, "wqkv")
            wp, wpB = P.sb([128, 8, 1280], BF16, "wqkp")
            S.dma("pool", w[:], attn_w_qkv[0].rearrange("(kc p) n -> p kc n", p=128), d, writes=[wB])
            S.dma("pool", wp[:], attn_w_qkp[0].rearrange("(kc p) n -> p kc n", p=128), d, writes=[wpB])
            rcs = [P.sb([64, 2, 512], F32, f"rc{i}") + (S.dsem(f"rc_{L}_{i}"),) for i in range(2)]
            qst = [P.sb([64, 20, 512], BF16, f"qst{i}") + (S.dsem(f"qst_{L}_{i}"),) for i in range(2)]
            vst = [P.sb([128, 4, 256], BF16, f"vst{i}") + (S.dsem(f"vst_{L}_{i}"),) for i in range(2)]
            t1 = P.sb([64, 512], F32, "t1")
            t2 = P.sb([64, 512], F32, "t2")
            return dict(w=w, wB=wB, wp=wp, wpB=wpB, rcs=rcs, qst=qst, vst=vst, t1=t1, t2=t2)

        def tile(P, c, ti, t0, ntok, hT, hTB):
            w, wB, wp, wpB = c["w"], c["wB"], c["wp"], c["wpB"]
            rc, rcB, rcd = c["rcs"][ti % 2]
            q, qB, qd = c["qst"][ti % 2]
            v, vB, vd = c["vst"][ti % 2]
            t1, t1B = c["t1"]
            t2, t2B = c["t2"]
            S.dma("sp", rc[:, 0, 0:ntok], ropec_in[:, t0:t0 + ntok], rcd, writes=[rcB])
            S.dma("sp", rc[:, 1, 0:ntok], ropes_in[:, t0:t0 + ntok], rcd, writes=[rcB])
            for hh in range(20):
                pq, pqB = nps()
                for kc in range(8):
                    S.op("pe", lambda e: e.matmul(pq[0:64, 0:ntok], w[:, kc, hh * 64:(hh + 1) * 64], hT[:, kc, 0:ntok], start=(kc == 0), stop=(kc == 7)),
                         reads=[wB, hTB], writes=[pqB], inc=(kc == 7))
                pp, ppB = nps()
                for kc in range(8):
                    S.op("pe", lambda e: e.matmul(pp[0:64, 0:ntok], wp[:, kc, hh * 64:(hh + 1) * 64], hT[:, kc, 0:ntok], start=(kc == 0), stop=(kc == 7)),
                         reads=[wpB, hTB], writes=[ppB], inc=(kc == 7))
                S.op("dve", lambda e: e.tensor_tensor(out=t1[:, 0:ntok], in0=pq[0:64, 0:ntok], in1=rc[:, 0, 0:ntok], op=ALU.mult), reads=[pqB, rcB], writes=[t1B])
                S.op("dve", lambda e: e.tensor_tensor(out=t2[:, 0:ntok], in0=pp[0:64, 0:ntok], in1=rc[:, 1, 0:ntok], op=ALU.mult), reads=[ppB, rcB], writes=[t2B])
                S.op("dve", lambda e: e.tensor_tensor(out=q[:, hh, 0:ntok], in0=t1[:, 0:ntok], in1=t2[:, 0:ntok], op=ALU.add), reads=[t1B, t2B], writes=[qB])
            for b in range(ntok // 128):
                pv, pvB = nps()
                for kc in range(8):
                    S.op("pe", lambda e: e.matmul(pv[:, 0:256], hT[:, kc, b * 128:(b + 1) * 128], w[:, kc, 1280:1536], start=(kc == 0), stop=(kc == 7)),
                         reads=[wB, hTB], writes=[pvB], inc=(kc == 7))
                S.op("act", lambda e: e.copy(out=v[:, b, :], in_=pv[:, 0:256]), reads=[pvB], writes=[vB])
            S.dma("sp", QT[:, :, t0:t0 + ntok].rearrange("h d t -> d h t"), q[:, 0:16, 0:ntok], qd, reads=[qB])
            S.dma("sp", KT[:, :, t0:t0 + ntok].rearrange("h d t -> d h t"), q[:, 16:20, 0:ntok], qd, reads=[qB])
            S.dma("sp", VV[t0:t0 + ntok, :].rearrange("(b p) c -> p b c", p=128), v[:, 0:ntok // 128, :], vd, reads=[vB])

        phase_norm_proj(L, setup, tile, last)
        if flags.get("swa_p1only"):
            return

        with Phase(S) as P:
            d = S.dsem(f"ak_{L}")
            KTs, KTB = P.sb([64, 4, T], BF16, "KTs")
            Vs, VB = P.sb([128, NBLK, 256], BF16, "Vs")
            mb, mbB = P.sb([128, 384], F32, "mb")
            sk, skB = P.sb([128, 16], F32, "sk")
            nsk, nskB = P.sb([128, 16], F32, "nsk")
            S.dma("sp", KTs[:], KT.rearrange("h d t -> d h t"), d, writes=[KTB])
            S.dma("sp", Vs[:], VV.rearrange("(b p) c -> p b c", p=128), d, writes=[VB])
            S.dma("sp", mb[:], maskb_in, d, writes=[mbB])
            S.dma("sp", sk[:], attn_sinks[0, :].partition_broadcast(128), d, writes=[skB])
            S.op("dve", lambda e: e.tensor_scalar(out=nsk[:], in0=sk[:], scalar1=-1.0, scalar2=None, op0=ALU.mult), reads=[skB], writes=[nskB])
            qts = [P.sb([64, 16, 128], BF16, f"qt{i}") + (S.dsem(f"qt_{L}_{i}"),) for i in range(2)]
            oto = [P.sb([128, D], BF16, f"oto{i}") for i in range(2)]
            ost = [P.sb([128, 8, 128], BF16, f"ost{i}") + (S.dsem(f"ost_{L}_{i}"),) for i in range(2)]
            Ssb = [P.sb([128, 640], F32, f"Ssb{i}") for i in range(2)]
            Pbs = [P.sb([128, 640], BF16, f"Pb{i}") for i in range(2)]
            PTs = [P.sb([128, 5, 128], BF16, f"PT{i}") for i in range(2)]
            sts = [P.sb([128, 8], F32, f"ast{i}") for i in range(2)]
            NQB = flags.get("swa_nqb", NBLK)
            items = [(qb, h) for qb in range(NQB) for h in range(16)]
            ctxs = {}

            def qb_info(qb):
                if qb < 2:
                    lks, m0 = [], 0
                else:
                    lq = qb - 2
                    lks = [x for x in (lq - 1, lq, lq + 1) if 0 <= x < 32]
                    m0 = 0 if lq - 1 >= 0 else 128
                nloc = len(lks) * 128
                return lks, m0, nloc, nloc + 256, [2 + x for x in lks] + [0, 1]

            def stage_a(i):
                qb, h = items[i]
                qt, qtB, qtd = qts[qb % 2]
                if h == 0:
                    S.dma("sp", qt[:], QT[:, :, qb * 128:(qb + 1) * 128].rearrange("h d t -> d h t"), qtd, writes=[qtB])
                lks, m0, nloc, n, vblks = qb_info(qb)
                kh = h // 4
                Sb, SbB = Ssb[i % 2]
                Pb, PbB = Pbs[i % 2]
                st_, stB_ = sts[i % 2]
                if nloc:
                    pa, paB = nps()
                    k0 = CTX + lks[0] * 128
                    S.op("pe", lambda e: e.matmul(pa[:, 0:nloc], qt[:, h, :], KTs[:, kh, k0:k0 + nloc], start=True, stop=True), reads=[qtB, KTB], writes=[paB])
                    S.op("dve", lambda e: e.tensor_tensor(out=Sb[:, 0:nloc], in0=pa[:, 0:nloc], in1=mb[:, m0:m0 + nloc], op=ALU.add), reads=[paB, mbB], writes=[SbB])
                pc, pcB = nps()
                S.op("pe", lambda e: e.matmul(pc[:, 0:256], qt[:, h, :], KTs[:, kh, 0:256], start=True, stop=True), reads=[qtB, KTB], writes=[pcB])
                S.op("act", lambda e: e.copy(out=Sb[:, nloc:n], in_=pc[:, 0:256]), reads=[pcB], writes=[SbB])
                S.op("dve", lambda e: e.reduce_max(out=st_[:, 0:1], in_=Sb[:, 0:n], axis=AX.X), reads=[SbB], writes=[stB_])
                S.op("dve", lambda e: e.tensor_scalar(out=st_[:, 1:2], in0=st_[:, 0:1], scalar1=-SC, scalar2=nsk[:, h:h + 1], op0=ALU.mult, op1=ALU.min), reads=[stB_, nskB], writes=[stB_])
                S.op("act", lambda e: e.activation(out=Pb[:, 0:n], in_=Sb[:, 0:n], func=AF.Exp, scale=SC, bias=st_[:, 1:2], accum_out=st_[:, 2:3]), reads=[SbB, stB_], writes=[PbB, stB_])
                S.op("act", lambda e: e.activation(out=st_[:, 3:4], in_=st_[:, 1:2], func=AF.Exp, bias=sk[:, h:h + 1]), reads=[stB_, skB], writes=[stB_])
                S.op("dve", lambda e: e.tensor_tensor(out=st_[:, 4:5], in0=st_[:, 2:3], in1=st_[:, 3:4], op=ALU.add), reads=[stB_], writes=[stB_])
                S.op("dve", lambda e: e.reciprocal(out=st_[:, 5:6], in_=st_[:, 4:5]), reads=[stB_], writes=[stB_])

            def stage_b(i):
                qb, h = items[i]
                lks, m0, nloc, n, vblks = qb_info(qb)
                kh = h // 4
                Pb, PbB = Pbs[i % 2]
                PT, PTB = PTs[i % 2]
                st_, stB_ = sts[i % 2]
                ot_, otB_ = oto[qb % 2]
                nkb = n // 128
                pt, ptB = nps()
                ptb = pt[:, :].bitcast(BF16)
                for kb in range(nkb):
                    S.op("pe", lambda e: e.transpose(ptb[:, kb * 128:(kb + 1) * 128], Pb[:, kb * 128:(kb + 1) * 128], identb[:]), reads=[PbB, identbB], writes=[ptB], inc=(kb == nkb - 1))
                S.op("act", lambda e: e.copy(out=PT[:, 0:nkb, :], in_=ptb[:, 0:n].rearrange("p (a b) -> p a b", a=nkb)), reads=[ptB], writes=[PTB])
                po, poB = nps()
                for kb in range(nkb):
                    S.op("pe", lambda e: e.matmul(po[:, 0:64], PT[:, kb, :], Vs[:, vblks[kb], kh * 64:(kh + 1) * 64], start=(kb == 0), stop=(kb == nkb - 1)),
                         reads=[PTB, VB], writes=[poB], inc=(kb == nkb - 1))
                S.op("dve", lambda e: e.tensor_scalar(out=ot_[:, h * 64:(h + 1) * 64], in0=po[:, 0:64], scalar1=st_[:, 5:6], scalar2=None, op0=ALU.mult), reads=[poB, stB_], writes=[otB_])
                if h == 15:
                    os_, osB, osd = ost[qb % 2]
                    pt, ptB = nps()
                    ptb = pt[:, :].bitcast(BF16)
                    for kc in range(8):
                        S.op("pe", lambda e: e.transpose(ptb[:, kc * 128:(kc + 1) * 128], ot_[:, kc * 128:(kc + 1) * 128], identb[:]), reads=[otB_, identbB], writes=[ptB], inc=(kc == 7))
                    S.op("act", lambda e: e.copy(out=os_[:], in_=ptb[:, :].rearrange("p (a b) -> p a b", a=8)), reads=[ptB], writes=[osB])
                    S.dma("sp", OT[:, :, qb * 128:(qb + 1) * 128].rearrange("j p t -> p j t"), os_[:], osd, reads=[osB], writes=otbuf)

            for i in range(len(items) + 1):
                if i < len(items):
                    stage_a(i)
                if i >= 1:
                    stage_b(i - 1)
        if not flags.get('no_outproj'):
            phase_outproj_norm2(L, attn_w_o[0], None, last, L // 2)


    def mixer_hyena(L, last):
        UH = dscr("UH", [24, 128, T])
        UT = dscr("UT", [3, T, D])
        Z1 = dscr("Z1", [T, D])
        HID = {256: dscr("HID256", [64, 256]), 4096: dscr("HID4096", [64, 4096])}
        APM = {Ln: dscr(f"APM{Ln}", [2, 2, 128, Ln // 128, D], BF16) for Ln in (256, 4096)}
        RIN = {Ln: dscr(f"RIN{Ln}", [2, D]) for Ln in (256, 4096)}
        KF = {Ln: dscr(f"KF{Ln}", [2, 2, (Ln // 128 + 1) * 128, D]) for Ln in (256, 4096)}
        YS = {Ln: dscr(f"YS{Ln}", [2, (Ln // 128 + 1) * 128, D], BF16) for Ln in (256, 4096)}
        zT = {256: zT256_in, 4096: zT4096_in}
        negt = {256: negt256_in, 4096: negt4096_in}
        FCm = {256: (FC256_in, FS256_in), 4096: (FC4096_in, FS4096_in)}
        GCm = {256: (GC256_in, GS256_in), 4096: (GC4096_in, GS4096_in)}
        TOK0 = {256: 0, 4096: CTX}
        PI = 3.1415925

        def setup(P):
            d = S.dsem(f"hw_{L}")
            w, wB = P.sb([128, 8, 3 * D], BF16, "hywin")
            for h in range(3):
                S.dma("pool", w[:, :, h * D:(h + 1) * D], hy_w_in[0, :, h * D:(h + 1) * D].rearrange("(kc p) n -> p kc n", p=128), d, writes=[wB])
            bi, biB = P.sb([128, 24], F32, "hybin")
            S.dma("sp", bi[:], hy_b_inT[0], d, writes=[biB])
            us = [P.sb([128, 12, 512], F32, f"hust{i}") + (S.dsem(f"hust_{L}_{i}"),) for i in range(2)]
            return dict(w=w, wB=wB, bi=bi, biB=biB, us=us, k=[0])

        def tile(P, c, ti, t0, ntok, hT, hTB):
            w, wB, bi, biB = c["w"], c["wB"], c["bi"], c["biB"]
            for hf in range(2):
                u, uB, ud = c["us"][c["k"][0] % 2]
                c["k"][0] += 1
                for jj in range(12):
                    ch = hf * 12 + jj
                    pu, puB = nps()
                    for kc in range(8):
                        S.op("pe", lambda e: e.matmul(pu[:, 0:ntok], w[:, kc, ch * 128:(ch + 1) * 128], hT[:, kc, 0:ntok], start=(kc == 0), stop=(kc == 7)),
                             reads=[wB, hTB], writes=[puB], inc=(kc == 7))
                    S.op("act", lambda e: e.activation(out=u[:, jj, 0:ntok], in_=pu[:, 0:ntok], func=AF.Identity, bias=bi[:, ch:ch + 1]), reads=[puB, biB], writes=[uB])
                S.dma("sp", UH[hf * 12:(hf + 1) * 12, :, t0:t0 + ntok].rearrange("j p t -> p j t"), u[:, :, 0:ntok], ud, reads=[uB])

        phase_norm_proj(L, setup, tile, last)

        with Phase(S) as P:
            d = S.dsem(f"hc_{L}")
            cw, cwB = P.sb([128, 24, 3], F32, "hcw")
            cb, cbB = P.sb([128, 24], F32, "hcb")
            S.dma("sp", cw[:], hy_conv_wT[0], d, writes=[cwB])
            S.dma("sp", cb[:], hy_conv_bT[0], d, writes=[cbB])
            ups = [P.sb([128, T], F32, f"hup{i}") + (S.dsem(f"hup_{L}_{i}"),) for i in range(2)]
            U, UB_ = P.sb([128, T], F32, "hU")
            tos = [P.sb([128, NBLK, 128], F32, f"hto{i}") + (S.dsem(f"hto_{L}_{i}"),) for i in range(2)]
            segs = [(0, CTX), (CTX, T)]
            for ch in range(24):
                UP, UPB, upd = ups[ch % 2]
                to, toB, tod = tos[ch % 2]
                S.dma("sp", UP[:], UH[ch, :, :], upd, writes=[UPB])
                S.op("act", lambda e: e.activation(out=U[:], in_=UP[:], func=AF.Identity, scale=cw[:, ch, 1:2], bias=cb[:, ch:ch + 1]), reads=[UPB, cwB, cbB], writes=[UB_])
                for (a, b_) in segs:
                    for (tap, sh) in ((0, -1), (2, 1)):
                        lo = max(a, a - sh)
                        hi = min(b_, b_ - sh)
                        S.op("dve", lambda e: e.scalar_tensor_tensor(out=U[:, lo:hi], in0=UP[:, lo + sh:hi + sh], scalar=cw[:, ch, tap:tap + 1], in1=U[:, lo:hi], op0=ALU.mult, op1=ALU.add),
                             reads=[UPB, cwB, UB_], writes=[UB_])
                for g4 in range(0, NBLK, 4):
                    nb4 = min(4, NBLK - g4)
                    pt, pB = nps()
                    for q in range(nb4):
                        S.op("pe", lambda e: e.transpose(pt[:, q * 128:(q + 1) * 128], U[:, (g4 + q) * 128:(g4 + q + 1) * 128], ident[:]), reads=[UB_, identB], writes=[pB], inc=(q == nb4 - 1))
                    S.op("act", lambda e: e.copy(out=to[:, g4:g4 + nb4, :], in_=pt[:, 0:nb4 * 128].rearrange("p (a b) -> p a b", a=nb4)), reads=[pB], writes=[toB])
                which, cc = ch // 8, ch % 8
                S.dma("sp", UT[which, :, cc * 128:(cc + 1) * 128].rearrange("(b p) c -> p b c", p=128), to[:], tod, reads=[toB])

        def range_reduce(arg, argB, ki, kiB, tmp, tmpB, n):
            S.op("dve", lambda e: e.tensor_scalar(out=ki[:, 0:n], in0=arg[:, 0:n], scalar1=1.0 / (2 * math.pi), scalar2=64.5, op0=ALU.mult, op1=ALU.add), reads=[argB], writes=[kiB])
            S.op("dve", lambda e: e.tensor_copy(out=tmp[:, 0:n], in_=ki[:, 0:n]), reads=[kiB], writes=[tmpB])
            S.op("dve", lambda e: e.tensor_scalar(out=tmp[:, 0:n], in0=tmp[:, 0:n], scalar1=-64.0, scalar2=-2 * math.pi, op0=ALU.add, op1=ALU.mult), reads=[tmpB], writes=[tmpB])
            S.op("dve", lambda e: e.tensor_tensor(out=arg[:, 0:n], in0=arg[:, 0:n], in1=tmp[:, 0:n], op=ALU.add), reads=[argB, tmpB], writes=[argB])
            S.op("dve", lambda e: e.tensor_scalar(out=tmp[:, 0:n], in0=arg[:, 0:n], scalar1=-PI, scalar2=2 * math.pi, op0=ALU.is_lt, op1=ALU.mult), reads=[argB], writes=[tmpB])
            S.op("dve", lambda e: e.tensor_tensor(out=arg[:, 0:n], in0=arg[:, 0:n], in1=tmp[:, 0:n], op=ALU.add), reads=[argB, tmpB], writes=[argB])
            S.op("dve", lambda e: e.tensor_scalar(out=tmp[:, 0:n], in0=arg[:, 0:n], scalar1=PI, scalar2=-2 * math.pi, op0=ALU.is_gt, op1=ALU.mult), reads=[argB], writes=[tmpB])
            S.op("dve", lambda e: e.tensor_tensor(out=arg[:, 0:n], in0=arg[:, 0:n], in1=tmp[:, 0:n], op=ALU.add), reads=[argB, tmpB], writes=[argB])
            S.op("dve", lambda e: e.tensor_scalar(out=arg[:, 0:n], in0=arg[:, 0:n], scalar1=-PI, scalar2=PI, op0=ALU.max, op1=ALU.min), reads=[argB], writes=[argB])

        with Phase(S) as P:
            d = S.dsem(f"hf_{L}")
            w1, w1B = P.sb([33, 64], F32, "fw1")
            w2, w2B = P.sb([64, 64], F32, "fw2")
            w3, w3B = P.sb([64, 64], F32, "fw3")
            fb, fbB = P.sb([64, 4], F32, "ffb")
            S.dma("sp", w1[:], hy_f_w1[0], d, writes=[w1B])
            S.dma("sp", w2[:], hy_f_w2[0], d, writes=[w2B])
            S.dma("sp", w3[:], hy_f_w3[0], d, writes=[w3B])
            for i, ap in enumerate((hy_f_b1, hy_f_b2, hy_f_b3, hy_f_freq)):
                S.dma("sp", fb[:, i:i + 1], ap[0], d, writes=[fbB])
            S.op("dve", lambda e: e.tensor_scalar(out=fb[:, 0:3], in0=fb[:, 0:3], scalar1=fb[:, 3:4], scalar2=None, op0=ALU.mult), reads=[fbB], writes=[fbB])
            zt, ztB = P.sb([33, 4096], F32, "zt")
            ha, haB = P.sb([64, 4096], F32, "ha")
            hb, hbB = P.sb([64, 4096], F32, "hb")
            ki, kiB = P.sb([64, 4096], I32, "ki")
            tmp, tmpB = P.sb([64, 4096], F32, "ftmp")
            for Ln in (256, 4096):
                S.dma("sp", zt[:, 0:Ln], zT[Ln], d, writes=[ztB])
                src, srcB, K_ = zt, ztB, 33
                for li, (w_, wB_) in enumerate(((w1, w1B), (w2, w2B), (w3, w3B))):
                    dst, dstB = (ha, haB) if li % 2 == 0 else (hb, hbB)
                    for c0 in range(0, Ln, 512):
                        n = min(512, Ln - c0)
                        pp, ppB = nps()
                        S.op("pe", lambda e: e.matmul(pp[0:64, 0:n], w_[0:K_, :], src[0:K_, c0:c0 + n], start=True, stop=True), reads=[wB_, srcB], writes=[ppB])
                        S.op("act", lambda e: e.activation(out=dst[:, c0:c0 + n], in_=pp[0:64, 0:n], func=AF.Identity, scale=fb[:, 3:4], bias=fb[:, li:li + 1]), reads=[ppB, fbB], writes=[dstB])
                    range_reduce(dst, dstB, ki, kiB, tmp, tmpB, Ln)
                    S.op("act", lambda e: e.activation(out=dst[:, 0:Ln], in_=dst[:, 0:Ln], func=AF.Sin), reads=[dstB], writes=[dstB])
                    src, srcB, K_ = dst, dstB, 64
                S.dma("sp", HID[Ln], src[:, 0:Ln], d, reads=[srcB])

        with Phase(S) as P:
            d = S.dsem(f"hg_{L}")
            w4, w4B = P.sb([64, 4096], F32, "fw4")
            S.dma("sp", w4[:], hy_f_w4[0], d, writes=[w4B])
            absd, absdB = P.sb([128, D], F32, "absd")
            S.dma("sp", absd[:], absd_in.partition_broadcast(128), d, writes=[absdB])
            ones, onesB = P.sb([128, 128], F32, "ones")
            S.dma("sp", ones[:], ones_in, d, writes=[onesB])
            hid, hidB = P.sb([64, 4096], F32, "hid")
            ngt, ngtB = P.sb([128, 32], F32, "ngt")
            dec, decB = P.sb([128, D], F32, "dec")
            hbk, hbkB = P.sb([128, 2, D], F32, "hbk")
            acc, accB = P.sb([128, 2, D], F32, "aacc")
            habs, habsB = P.sb([128, 2, D], F32, "habs")
            nrm, nrmB = P.sb([128, 2, D], F32, "nrm")
            apms = [P.sb([128, 2, D], BF16, f"apm{i}") + (S.dsem(f"apm_{L}_{i}"),) for i in range(2)]
            k = 0
            for Ln in (256, 4096):
                nb = Ln // 128
                S.dma("sp", hid[:, 0:Ln], HID[Ln], d, writes=[hidB])
                S.dma("sp", ngt[:, 0:nb], negt[Ln], d, writes=[ngtB])
                for o in range(2):
                    for blk in range(nb):
                        S.op("act", lambda e: e.activation(out=dec[:], in_=absd[:], func=AF.Exp, scale=ngt[:, blk:blk + 1]), reads=[absdB, ngtB], writes=[decB])
                        for cg in range(4):
                            pp, ppB = nps()
                            S.op("pe", lambda e: e.matmul(pp[:, :], hid[:, blk * 128:(blk + 1) * 128], w4[:, o * 2048 + cg * 512:o * 2048 + (cg + 1) * 512], start=True, stop=True), reads=[hidB, w4B], writes=[ppB])
                            sd_, hf = cg // 2, cg % 2
                            S.op("dve", lambda e: e.tensor_tensor(out=hbk[:, sd_, hf * 512:(hf + 1) * 512], in0=pp[:, :], in1=dec[:, hf * 512:(hf + 1) * 512], op=ALU.mult), reads=[ppB, decB], writes=[hbkB])
                        if blk == 0:
                            S.op("dve", lambda e: e.memset(hbk[0:1, 1, :], 0.0), writes=[hbkB])
                            S.op("act", lambda e: e.activation(out=acc[:], in_=hbk[:], func=AF.Abs), reads=[hbkB], writes=[accB])
                        else:
                            S.op("act", lambda e: e.activation(out=habs[:], in_=hbk[:], func=AF.Abs), reads=[hbkB], writes=[habsB])
                            S.op("dve", lambda e: e.tensor_tensor(out=acc[:], in0=acc[:], in1=habs[:], op=ALU.add), reads=[habsB, accB], writes=[accB])
                        apm, apmB, apmd = apms[k % 2]
                        k += 1
                        S.op("dve", lambda e: e.tensor_tensor(out=apm[:, 0, :], in0=hbk[:, 0, :], in1=hbk[:, 1, :], op=ALU.add), reads=[hbkB], writes=[apmB])
                        S.op("dve", lambda e: e.tensor_tensor(out=apm[:, 1, :], in0=hbk[:, 1, :], in1=hbk[:, 0, :], op=ALU.subtract), reads=[hbkB], writes=[apmB])
                        S.dma("sp", APM[Ln][o, :, :, blk, :].rearrange("s p c -> p s c"), apm[:], apmd, reads=[apmB])
                    for cg in range(4):
                        pp, ppB = nps()
                        sd_, hf = cg // 2, cg % 2
                        S.op("pe", lambda e: e.matmul(pp[:, :], ones[:], acc[:, sd_, hf * 512:(hf + 1) * 512], start=True, stop=True), reads=[onesB, accB], writes=[ppB])
                        S.op("act", lambda e: e.copy(out=nrm[:, sd_, hf * 512:(hf + 1) * 512], in_=pp[:, :]), reads=[ppB], writes=[nrmB])
                    S.op("dve", lambda e: e.tensor_tensor(out=nrm[:, 0, :], in0=nrm[:, 0, :], in1=nrm[:, 1, :], op=ALU.add), reads=[nrmB], writes=[nrmB])
                    S.op("dve", lambda e: e.reciprocal(out=nrm[:, 1, :], in_=nrm[:, 0, :]), reads=[nrmB], writes=[nrmB])
                    S.dma("sp", RIN[Ln][o:o + 1, :], nrm[0:1, 1, :], d, reads=[nrmB])

        def fwd_dft(P, Ln, R1, R1B, R2, R2B, emit, tag):
            nb, nfc = Ln // 128, Ln // 128 + 1
            FC_, FS_ = FCm[Ln]
            fts = [P.sb([128, nb, 128], BF16, f"ft{tag}{i}") + (S.dsem(f"ft_{tag}_{i}"),) for i in range(4)]
            def _ld(fc):
                a, aB, ad = fts[(2 * fc) % 4]
                b, bB, bd = fts[(2 * fc + 1) % 4]
                S.dma("sp", a[:], FC_[fc], ad, writes=[aB])
                S.dma("sp", b[:], FS_[fc], bd, writes=[bB])
            _ld(0)
            for fc in range(nfc):
                fct, fcB, fcd = fts[(2 * fc) % 4]
                fst, fsB, fsd = fts[(2 * fc + 1) % 4]
                if fc + 1 < nfc:
                    _ld(fc + 1)
                for half in range(2):
                    pc, pcB = nps()
                    for blk in range(nb):
                        S.op("pe", lambda e: e.matmul(pc[:, :], fct[:, blk, :], R1[:, blk, half * 512:(half + 1) * 512], start=(blk == 0), stop=(blk == nb - 1)),
                             reads=[fcB, R1B], writes=[pcB], inc=(blk == nb - 1))
                    pS, pSB = nps()
                    for blk in range(nb):
                        S.op("pe", lambda e: e.matmul(pS[:, :], fst[:, blk, :], R2[:, blk, half * 512:(half + 1) * 512], start=(blk == 0), stop=(blk == nb - 1)),
                             reads=[fsB, R2B], writes=[pSB], inc=(blk == nb - 1))
                    emit(fc, half, pc, pcB, pS, pSB)

        for Ln in (256, 4096):
            nb = Ln // 128
            for o in range(2):
                with Phase(S) as P:
                    d = S.dsem(f"hk_{L}_{Ln}_{o}")
                    Ap, ApB = P.sb([128, nb, D], BF16, "Ap")
                    Am, AmB = P.sb([128, nb, D], BF16, "Am")
                    S.dma("sp", Ap[:], APM[Ln][o, 0], d, writes=[ApB])
                    S.dma("sp", Am[:], APM[Ln][o, 1], d, writes=[AmB])
                    rin, rinB = P.sb([128, D], F32, "rin")
                    S.dma("sp", rin[:], RIN[Ln][o, :].partition_broadcast(128), d, writes=[rinB])
                    kos = [P.sb([128, 2, 512], F32, f"ko{i}") + (S.dsem(f"ko_{L}_{Ln}_{o}_{i}"),) for i in range(2)]
                    cnt = [0]

                    def emit(fc, half, pc, pcB, pS, pSB):
                        ko, koB, kod = kos[cnt[0] % 2]
                        cnt[0] += 1
                        hs = slice(half * 512, (half + 1) * 512)
                        S.op("dve", lambda e: e.tensor_tensor(out=ko[:, 0, :], in0=pc[:, :], in1=rin[:, hs], op=ALU.mult), reads=[pcB, rinB], writes=[koB])
                        S.op("dve", lambda e: e.tensor_tensor(out=ko[:, 1, :], in0=pS[:, :], in1=rin[:, hs], op=ALU.mult), reads=[pSB, rinB], writes=[koB])
                        S.dma("act", KF[Ln][o, :, fc * 128:(fc + 1) * 128, hs].rearrange("r p c -> p r c"), ko[:], kod, reads=[koB])

                    fwd_dft(P, Ln, Ap, ApB, Am, AmB, emit, f"k{Ln}{o}")

        for o in range(2):
            for Ln in (256, 4096):
                nb, nfc = Ln // 128, Ln // 128 + 1
                tk0 = TOK0[Ln]
                src = UT[2] if o == 0 else Z1
                with Phase(S) as P:
                    d = S.dsem(f"hy_{L}_{o}_{Ln}")
                    R, RB_ = P.sb([128, nb, D], BF16, "R")
                    S.dma("pool", R[:], src[tk0:tk0 + Ln, :].rearrange("(b p) c -> p b c", p=128), d, writes=[RB_])
                    kts = [P.sb([128, 2, 512], F32, f"kt{i}") + (S.dsem(f"kt_{L}_{o}_{Ln}_{i}"),) for i in range(2)]
                    yos = [P.sb([128, 2, 512], BF16, f"yo{i}") + (S.dsem(f"yo_{L}_{o}_{Ln}_{i}"),) for i in range(2)]
                    t1, t1B = P.sb([128, 512], F32, "yt1")
                    t2, t2B = P.sb([128, 512], F32, "yt2")
                    cnt = [0]

                    def emit(fc, half, pc, pcB, pS, pSB):
                        kt, ktB, ktd = kts[cnt[0] % 2]
                        yo, yoB, yod = yos[cnt[0] % 2]
                        cnt[0] += 1
                        hs = slice(half * 512, (half + 1) * 512)
                        S.dma("sp", kt[:], KF[Ln][o, :, fc * 128:(fc + 1) * 128, hs].rearrange("r p c -> p r c"), ktd, writes=[ktB])
                        S.op("dve", lambda e: e.tensor_tensor(out=t1[:], in0=pc[:, :], in1=kt[:, 0, :], op=ALU.mult), reads=[pcB, ktB], writes=[t1B])
                        S.op("dve", lambda e: e.tensor_tensor(out=t2[:], in0=pS[:, :], in1=kt[:, 1, :], op=ALU.mult), reads=[pSB, ktB], writes=[t2B])
                        S.op("dve", lambda e: e.tensor_tensor(out=yo[:, 0, :], in0=t1[:], in1=t2[:], op=ALU.add), reads=[t1B, t2B], writes=[yoB])
                        S.op("dve", lambda e: e.tensor_tensor(out=t1[:], in0=pc[:, :], in1=kt[:, 1, :], op=ALU.mult), reads=[pcB, ktB], writes=[t1B])
                        S.op("dve", lambda e: e.tensor_tensor(out=t2[:], in0=pS[:, :], in1=kt[:, 0, :], op=ALU.mult), reads=[pSB, ktB], writes=[t2B])
                        S.op("dve", lambda e: e.tensor_tensor(out=yo[:, 1, :], in0=t1[:], in1=t2[:], op=ALU.subtract), reads=[t1B, t2B], writes=[yoB])
                        S.dma("act", YS[Ln][:, fc * 128:(fc + 1) * 128, hs].rearrange("r p c -> p r c"), yo[:], yod, reads=[yoB])

                    fwd_dft(P, Ln, R, RB_, R, RB_, emit, f"y{Ln}{o}")

                with Phase(S) as P:
                    d = S.dsem(f"hi_{L}_{o}_{Ln}")
                    Yr, YrB = P.sb([128, nfc, D], BF16, "Yr")
                    Yi, YiB = P.sb([128, nfc, D], BF16, "Yi")
                    S.dma("sp", Yr[:], YS[Ln][0].rearrange("(f p) c -> p f c", p=128), d, writes=[YrB])
                    S.dma("sp", Yi[:], YS[Ln][1].rearrange("(f p) c -> p f c", p=128), d, writes=[YiB])
                    skp, skpB = P.sb([128, D], F32, "skp")
                    S.dma("sp", skp[:], hy_skip[0, o, :].partition_broadcast(128), d, writes=[skpB])
                    GC_, GS_ = GCm[Ln]
                    gts = [P.sb([128, nfc, 128], BF16, f"gt{i}") + (S.dsem(f"gt_{L}_{o}_{Ln}_{i}"),) for i in range(4)]
                    ins = [P.sb([128, D], F32, f"in{i}") + (S.dsem(f"in_{L}_{o}_{Ln}_{i}"),) for i in range(2)]
                    ggs = [P.sb([128, D], F32, f"gg{i}") + (S.dsem(f"gg_{L}_{o}_{Ln}_{i}"),) for i in range(2)]
                    zos = [P.sb([128, D], F32, f"zo{i}") + (S.dsem(f"zo_{L}_{o}_{Ln}_{i}"),) for i in range(2)]
                    osts = [P.sb([128, 8, 128], BF16, f"hos{i}") + (S.dsem(f"hos_{L}_{o}_{Ln}_{i}"),) for i in range(2)]
                    def _ldi(tb):
                        a, aB, ad = gts[(2 * tb) % 4]
                        b, bB, bd = gts[(2 * tb + 1) % 4]
                        S.dma("sp", a[:], GC_[tb], ad, writes=[aB])
                        S.dma("sp", b[:], GS_[tb], bd, writes=[bB])
                        i_, iB, idd = ins[tb % 2]
                        g_, gB_, gdd = ggs[tb % 2]
                        r0_ = tk0 + tb * 128
                        S.dma("sp", i_[:], src[r0_:r0_ + 128, :], idd, writes=[iB])
                        S.dma("sp", g_[:], UT[o, r0_:r0_ + 128, :], gdd, writes=[gB_])
                    _ldi(0)
                    for tb in range(nb):
                        gct, gcB, gcd = gts[(2 * tb) % 4]
                        gst, gsB, gsd = gts[(2 * tb + 1) % 4]
                        it_, itB, itd = ins[tb % 2]
                        gg, ggB, ggd = ggs[tb % 2]
                        zo, zoB, zod = zos[tb % 2]
                        r0 = tk0 + tb * 128
                        if tb + 1 < nb:
                            _ldi(tb + 1)
                        S.op("dve", lambda e: e.tensor_tensor(out=zo[:], in0=it_[:], in1=skp[:], op=ALU.mult), reads=[itB, skpB], writes=[zoB])
                        for half in range(2):
                            hs = slice(half * 512, (half + 1) * 512)
                            py, pyB = nps()
                            for fc in range(nfc):
                                S.op("pe", lambda e: e.matmul(py[:, :], gct[:, fc, :], Yr[:, fc, hs], start=(fc == 0), stop=False), reads=[gcB, YrB], writes=[pyB], inc=False)
                            for fc in range(nfc):
                                S.op("pe", lambda e: e.matmul(py[:, :], gst[:, fc, :], Yi[:, fc, hs], start=False, stop=(fc == nfc - 1)), reads=[gsB, YiB], writes=[pyB], inc=(fc == nfc - 1))
                            S.op("dve", lambda e: e.tensor_tensor(out=zo[:, hs], in0=zo[:, hs], in1=py[:, :], op=ALU.add), reads=[zoB, pyB], writes=[zoB])
                        S.op("dve", lambda e: e.tensor_tensor(out=zo[:], in0=zo[:], in1=gg[:], op=ALU.mult), reads=[zoB, ggB], writes=[zoB])
                        if o == 0:
                            S.dma("act", Z1[r0:r0 + 128, :], zo[:], zod, reads=[zoB])
                        else:
                            os_, osB, osd = osts[tb % 2]
                            for half in range(2):
                                pt, pB = nps()
                                for q in range(4):
                                    kc = half * 4 + q
                                    S.op("pe", lambda e: e.transpose(pt[:, q * 128:(q + 1) * 128], zo[:, kc * 128:(kc + 1) * 128], ident[:]), reads=[zoB, identB], writes=[pB], inc=(q == 3))
                                S.op("act", lambda e: e.copy(out=os_[:, half * 4:half * 4 + 4, :], in_=pt[:, :].rearrange("p (a b) -> p a b", a=4)), reads=[pB], writes=[osB])
                            S.dma("act", OT[:, :, r0:r0 + 128].rearrange("j p t -> p j t"), os_[:], osd, reads=[osB], writes=otbuf)
        phase_outproj_norm2(L, hy_w_out[0], hy_b_out[0, :], last, None)

    for L, part in segs:
        last = (L == 3)
        mk = L % 3
        if flags.get("skip_mixer") or part == "ffn":
            pass
        elif mk == 0:
            mixer_rglru(L, L // 3, last)
        elif mk == 1:
            mixer_swa(L, last)
        else:
            mixer_hyena(L, last)
        if not flags.get("skip_ffn") and part != "mix":
            phase_ffn(L, L // 2 if L % 2 == 0 else None, L // 2 if L % 2 == 1 else None, last)

    with Phase(S) as P:
        fd = S.dsem("fin")
        S.dma("sp", xs_out, XS[:, :], fd, reads=xsbuf)
        gfin, gfinB = P.sb([128, D], F32, "gfin")
        S.dma("sp", gfin[:], norm_final.partition_broadcast(128), fd, writes=[gfinB])
        xsl = [P.sb([128, D], F32, f"x{i}") + (S.dsem(f"xf_{i}"),) for i in range(3)]
        junk, junkB = P.sb([128, D], BF16, "junk")
        st = P.sb([128, 4], F32, "st")
        for blk in range(2, NBLK):
            xt, xB, xd = xsl[blk % 3]
            S.dma("sp", xt[:], XS[blk * 128:(blk + 1) * 128, :], xd, reads=[xsbuf[blk]], writes=[xB])
            rms_rstd(P, xt, xB, junk, junkB, st)
            S.op("dve", lambda e, xt=xt: e.scalar_tensor_tensor(out=xt[:], in0=xt[:], scalar=st[0][:, 2:3], in1=gfin[:], op0=ALU.mult, op1=ALU.mult),
                 reads=[xB, st[1], gfinB], writes=[xB])
            S.dma("sp", out[(blk - 2) * 128:(blk - 1) * 128, :], xt[:], xd, reads=[xB])
    G.__exit__()
    es.close()
    return nc, S


_COMMON = ["xs_in", "c2T", "ada_w", "ada_b", "norm_mix", "norm_ffn", "norm_final", "ident", "identb"]
_LRU = ["lru_w_in", "lru_conv_wT", "lru_conv_bT", "lru_w_a", "lru_b_aT", "lru_w_x", "lru_b_xT", "lru_lamT", "lru_w_out"]
_SWA = ["attn_w_qkv", "attn_w_qkp", "attn_sinks", "attn_w_o", "maskb", "ropec", "ropes"]
_HY = ["hy_w_in", "hy_b_inT", "hy_conv_wT", "hy_conv_bT", "hy_f_w1", "hy_f_b1", "hy_f_w2", "hy_f_b2", "hy_f_w3", "hy_f_b3", "hy_f_freq", "hy_f_w4",
       "hy_skip", "hy_w_out", "hy_b_out", "ones", "absd", "zT256", "zT4096", "negt256", "negt4096",
       "FC256", "FS256", "GC256", "GS256", "FC4096", "FS4096", "GC4096", "GS4096"]
_DENSE = ["ffn_w_gate", "ffn_w_up", "ffn_w_down"]
_MOE = ["moe_router", "moe_w_gate", "moe_w_up", "moe_w_down"]


def _needed(segs):
    n = set(_COMMON)
    for sg in segs:
        L, part = (sg, "all") if isinstance(sg, int) else sg
        if part != "ffn":
            n |= set((_LRU, _SWA, _HY)[L % 3])
            if L % 2 == 1:
                n.add("moe_router")
        if part != "mix":
            n |= set(_DENSE if L % 2 == 0 else _MOE)
        if part == "ffn":
            n |= {"h2t_in", "gts_in"}
    return n


def _consts():
    c = {}
    c["ident"] = np.eye(128, dtype=np.float32)
    c["identb"] = np.eye(128, dtype=np.float32).astype(ml_dtypes.bfloat16)
    q = np.arange(128)[:, None]
    jk = np.arange(128)[None, :]
    m = np.zeros((128, 384), np.float32)
    m[:, 0:128] = np.where(jk >= q, 0.0, -1e30)
    m[:, 256:384] = np.where(jk <= q, 0.0, -1e30)
    c["maskb"] = m
    qd = 16
    inv = (10000.0 ** (-np.arange(qd, dtype=np.float32) / qd)).astype(np.float32)
    rows = SEQ // 64
    row = np.repeat(np.arange(rows, dtype=np.float32), 64)
    col = np.tile(np.arange(64, dtype=np.float32), rows)
    ang = np.concatenate([row[:, None] * inv, col[:, None] * inv], axis=-1).astype(np.float32)
    cos, sin = np.cos(ang), np.sin(ang)
    rc = np.ones((64, T), np.float32)
    rs = np.zeros((64, T), np.float32)
    for d in range(64):
        half = 0 if d < 32 else 16
        rc[d, CTX:] = cos[:, half + d % 16]
        sgn = -1.0 if (d % 32) < 16 else 1.0
        rs[d, CTX:] = sgn * sin[:, half + d % 16]
    c["ropec"], c["ropes"] = rc, rs
    c["ones"] = np.ones((128, 128), np.float32)
    deltas = np.linspace(math.log(1e-2) / 1.5, math.log(1e-2) / 0.3, D, dtype=np.float32)
    c["absd"] = np.abs(deltas).astype(np.float32)
    for Ln in (256, 4096):
        nb, nfc = Ln // 128, Ln // 128 + 1
        t = np.linspace(0.0, 1.0, Ln, dtype=np.float32)[:, None]
        omega = ((2.0 * math.pi / Ln) * np.arange(Ln, dtype=np.float32))[:, None].astype(np.float32)
        bands = np.linspace(1e-4, 15, 16, dtype=np.float32)[None, :]
        z = np.concatenate([t, np.cos(bands * omega), -np.sin(bands * omega)], axis=-1).astype(np.float32)
        c[f"zT{Ln}"] = np.ascontiguousarray(z.T)
        c[f"negt{Ln}"] = np.ascontiguousarray((-t[:, 0]).reshape(nb, 128).T)
        N2 = 2 * Ln
        tt = np.arange(Ln, dtype=np.int64)
        kk = np.arange(nfc * 128, dtype=np.int64)
        ang = (2.0 * np.pi / N2) * ((tt[:, None] * kk[None, :]) % N2).astype(np.float64)
        valid = (kk <= Ln).astype(np.float64)[None, :]
        Cm = np.cos(ang) * valid
        Sm = np.sin(ang) * valid
        c[f"FC{Ln}"] = np.ascontiguousarray(Cm.reshape(nb, 128, nfc, 128).transpose(2, 1, 0, 3)).astype(ml_dtypes.bfloat16)
        c[f"FS{Ln}"] = np.ascontiguousarray(Sm.reshape(nb, 128, nfc, 128).transpose(2, 1, 0, 3)).astype(ml_dtypes.bfloat16)
        wk = np.where((kk == 0) | (kk == Ln), 1.0, 2.0) * (kk <= Ln) / N2
        Gc = (Cm * wk[None, :]).T
        Gs = (-Sm * wk[None, :]).T
        c[f"GC{Ln}"] = np.ascontiguousarray(Gc.reshape(nfc, 128, nb, 128).transpose(2, 1, 0, 3)).astype(ml_dtypes.bfloat16)
        c[f"GS{Ln}"] = np.ascontiguousarray(Gs.reshape(nfc, 128, nb, 128).transpose(2, 1, 0, 3)).astype(ml_dtypes.bfloat16)
    return c


_CONSTS = None


def _prep(inputs, b):
    global _CONSTS
    if _CONSTS is None:
        _CONSTS = _consts()
    f = lambda a: np.ascontiguousarray(a, dtype=np.float32)
    m = {}
    m["xs_in"] = f(np.concatenate([inputs["ctx"][b], inputs["x"][b]], axis=0))
    c2 = np.stack([inputs["c"][b], inputs["c_ctx"]], axis=-1)
    m["c2T"] = f(c2.reshape(8, 128, 2).transpose(1, 0, 2))
    for k in ("ada_w", "ada_b", "norm_mix", "norm_ffn", "norm_final", "lru_w_in", "lru_w_a", "lru_w_x", "lru_w_out", "attn_w_qkv", "attn_sinks", "attn_w_o",
              "hy_w_in", "hy_f_w1", "hy_f_w2", "hy_f_w3", "hy_f_w4", "hy_skip", "hy_w_out", "hy_b_out", "ffn_w_gate", "ffn_w_up", "ffn_w_down",
              "moe_router", "moe_w_gate", "moe_w_up", "moe_w_down"):
        m[k] = f(inputs[k])
    m["lru_conv_wT"] = f(inputs["lru_conv_w"].reshape(2, 4, 8, 128).transpose(0, 3, 2, 1))
    m["lru_conv_bT"] = f(inputs["lru_conv_b"].reshape(2, 8, 128).transpose(0, 2, 1))
    for k in ("lru_b_a", "lru_b_x"):
        m[k + "T"] = f(inputs[k].reshape(2, 2, 8, 128).transpose(0, 1, 3, 2))
    m["lru_lamT"] = f(inputs["lru_lambda"].reshape(2, 2, 8, 128).transpose(0, 1, 3, 2))
    perm = np.concatenate([np.arange(16, 32), np.arange(0, 16), np.arange(48, 64), np.arange(32, 48)])
    cols = np.concatenate([h * 64 + perm for h in range(20)])
    m["attn_w_qkp"] = f(inputs["attn_w_qkv"][:, :, cols])
    m["hy_b_inT"] = f(inputs["hy_b_in"].reshape(1, 24, 128).transpose(0, 2, 1))
    m["hy_conv_wT"] = f(inputs["hy_conv_w"].reshape(1, 3, 24, 128).transpose(0, 3, 2, 1))
    m["hy_conv_bT"] = f(inputs["hy_conv_b"].reshape(1, 24, 128).transpose(0, 2, 1))
    for k in ("hy_f_b1", "hy_f_b2", "hy_f_b3", "hy_f_freq"):
        m[k] = f(inputs[k].reshape(1, 64, 1))
    m.update(_CONSTS)
    return m


def kernel(**inputs):
    maps = [_prep(inputs, b) for b in range(8)]
    outs = None
    for seg in SEGMENTS:
        nc, S = build(seg)
        need = _needed(seg)
        in_maps = [{k: v for k, v in m.items() if k in need} for m in maps]
        res = run_bass_kernel_spmd(nc, in_maps, core_ids=list(range(8)))
        for b in range(8):
            r = res.results[b]
            maps[b]["xs_in"] = np.asarray(r["xs_out"], dtype=np.float32)
            if "h2t_out" in r:
                maps[b]["h2t_in"] = np.asarray(r["h2t_out"])
                maps[b]["gts_in"] = np.asarray(r["gts_out"], dtype=np.float32)
        outs = [np.asarray(r["out"], dtype=np.float32) for r in res.results]
    return np.stack(outs, axis=0)


SEGMENTS = [[(0, "all"), (1, "all"), (2, "all"), (3, "all")]]
```

```python
import math
from contextlib import ExitStack
import numpy as np
import ml_dtypes
import concourse.bass as bass
import concourse.mybir as mybir
from concourse.bass_utils import run_bass_kernel_spmd

F32, BF16, I32 = mybir.dt.float32, mybir.dt.bfloat16, mybir.dt.int32
AF = mybir.ActivationFunctionType
ALU = mybir.AluOpType
AX = mybir.AxisListType

D = 1024
SEQ = 4096
CTX = 256
T = SEQ + CTX
NBLK = T // 128
DFF = 2816
DFE = 3584
NE = 8
EPS = 1e-6
TILES = [(0, 256)] + [(256 + 512 * i, 512) for i in range(8)]


class DSem:
    def __init__(s, sem, name):
        s.sem, s.name, s.cnt = sem, name, 0
        s.twin = None


class Tok:
    __slots__ = ("eng", "val", "dma")

    def __init__(s, eng, val, dma):
        s.eng, s.val, s.dma = eng, val, dma


class Buf:
    def __init__(s, name="", psum=False):
        s.name, s.w, s.r, s.psum = name, None, {}, psum


class Sched:
    NO_RECYCLE = False
    STRICT = True

    def __init__(s, nc, es):
        s.nc, s.es = nc, es
        s.E = dict(pe=nc.tensor, act=nc.scalar, dve=nc.vector, pool=nc.gpsimd, sp=nc.sync)
        s.psem = {e: es.enter_context(nc.semaphore("p_" + e)) for e in s.E}
        s.pcnt = {e: 0 for e in s.E}
        s.pend = {e: False for e in s.E}
        s.seen = {e: {} for e in s.E}
        s.dsems = []
        s.free_ds = []
        s.phase_stack = []
        s.nwait = 0

    def dsem(s, name):
        if s.free_ds and not Sched.NO_RECYCLE:
            d = s.free_ds.pop()
        else:
            d = DSem(s.es.enter_context(s.nc.semaphore("d_" + name)), name)
            s.dsems.append(d)
        if s.phase_stack:
            s.phase_stack[-1].dsems.append(d)
        return d

    def _wait(s, e, sem, key, val):
        if s.seen[e].get(key, 0) >= val:
            return
        s.E[e].wait_ge(sem, val)
        s.seen[e][key] = val
        s.nwait += 1

    def _dep(s, e, t, raw):
        if t.dma is not None:
            s._wait(e, t.dma.sem, "d_" + t.dma.name, t.dma.cnt)
            return
        if t.eng == e:
            if e == "pe":
                return
            if e != "pool" and not Sched.STRICT and not (raw and t.val >= s.pcnt[e]):
                return
        s._wait(e, s.psem[t.eng], "p_" + t.eng, t.val)

    def _deps(s, e, reads, writes):
        for b in reads:
            if b.w is not None:
                s._dep(e, b.w, True)
            if b.psum:
                for t in b.r.values():
                    s._dep(e, t, False)
        for b in writes:
            if b.w is not None:
                s._dep(e, b.w, False)
            for t in b.r.values():
                s._dep(e, t, False)

    def op(s, e, fn, reads=(), writes=(), inc=True):
        s._deps(e, reads, writes)
        inst = fn(s.E[e])
        if inc:
            s.pcnt[e] += 1
            inst.then_inc(s.psem[e], 1)
            t = Tok(e, s.pcnt[e], None)
            s.pend[e] = False
        else:
            t = Tok(e, s.pcnt[e] + 1, None)
            s.pend[e] = True
        for b in reads:
            b.r["p_" + e] = t
        for b in writes:
            b.w = t
            b.r = {}

    def dma(s, q, out, in_, ds, reads=(), writes=(), **kw):
        if q == "pool":
            if ds.twin is None:
                ds.twin = DSem(s.es.enter_context(s.nc.semaphore("w_" + ds.name)), "sw_" + ds.name)
                s.dsems.append(ds.twin)
            ds = ds.twin
        s._deps(q, reads, writes)
        inst = s.E[q].dma_start(out=out, in_=in_, **kw)
        ds.cnt += 16
        inst.then_inc(ds.sem, 16)
        t = Tok(q, ds.cnt, ds)
        for b in reads:
            b.r["d_" + ds.name] = t
        for b in writes:
            b.w = t
            b.r = {}

    def barrier(s):
        for e in s.E:
            assert not s.pend[e], e
        for e in s.E:
            for e2 in s.E:
                if e2 != e and s.pcnt[e2] > 0:
                    s._wait(e, s.psem[e2], "p_" + e2, s.pcnt[e2])
            for d in s.dsems:
                if d.cnt:
                    s._wait(e, d.sem, "d_" + d.name, d.cnt)


class Phase:
    CNT = 0

    def __init__(s, S):
        s.S, s.es = S, ExitStack()
        s.dsems = []
        S.phase_stack.append(s)

    def __enter__(s):
        return s

    def sb(s, shape, dt, name=None):
        Phase.CNT += 1
        t = s.es.enter_context(s.S.nc.sbuf_tensor(f"{name or 't'}_{Phase.CNT}", list(shape), dt))
        return t, Buf(name or "sb")

    def __exit__(s, *a):
        s.S.barrier()
        s.es.close()
        assert s.S.phase_stack.pop() is s
        s.S.free_ds.extend(s.dsems)
        return False


def build(segs=((0, "all"), (1, "all"), (2, "all"), (3, "all")), flags=None):
    flags = flags or {}
    Sched.NO_RECYCLE = bool(flags.get("no_recycle"))
    segs = [(sg, "all") if isinstance(sg, int) else tuple(sg) for sg in segs]
    layers = [L for L, _ in segs]
    parts = {p for _, p in segs}
    need = _needed(segs)
    nc = bass.Bass("TRN2", target_bir_lowering=False)
    es = ExitStack()
    S = Sched(nc, es)

    def din(name, shape, dt=F32):
        if name not in need:
            return None
        return nc.dram_tensor(name, list(shape), dt, kind="ExternalInput").ap()

    def dscr(name, shape, dt=F32):
        return nc.dram_tensor(name, list(shape), dt, kind="Internal").ap()

    xs_in = din("xs_in", [T, D])
    c2T = din("c2T", [128, 8, 2])
    ada_w = din("ada_w", [4, D, 6 * D])
    ada_b = din("ada_b", [4, 6 * D])
    norm_mix = din("norm_mix", [4, D])
    norm_ffn = din("norm_ffn", [4, D])
    norm_final = din("norm_final", [D])
    lru_w_in = din("lru_w_in", [2, D, 2 * D])
    lru_conv_wT = din("lru_conv_wT", [2, 128, 8, 4])
    lru_conv_bT = din("lru_conv_bT", [2, 128, 8])
    lru_w_a = din("lru_w_a", [2, 2, 8, 128, 128])
    lru_b_aT = din("lru_b_aT", [2, 2, 128, 8])
    lru_w_x = din("lru_w_x", [2, 2, 8, 128, 128])
    lru_b_xT = din("lru_b_xT", [2, 2, 128, 8])
    lru_lamT = din("lru_lamT", [2, 2, 128, 8])
    lru_w_out = din("lru_w_out", [2, D, D])
    attn_w_qkv = din("attn_w_qkv", [1, D, 1536])
    attn_w_qkp = din("attn_w_qkp", [1, D, 1280])
    attn_sinks = din("attn_sinks", [1, 16])
    attn_w_o = din("attn_w_o", [1, D, D])
    hy_w_in = din("hy_w_in", [1, D, 3 * D])
    hy_b_inT = din("hy_b_inT", [1, 128, 24])
    hy_conv_wT = din("hy_conv_wT", [1, 128, 24, 3])
    hy_conv_bT = din("hy_conv_bT", [1, 128, 24])
    hy_f_w1 = din("hy_f_w1", [1, 33, 64])
    hy_f_b1 = din("hy_f_b1", [1, 64, 1])
    hy_f_w2 = din("hy_f_w2", [1, 64, 64])
    hy_f_b2 = din("hy_f_b2", [1, 64, 1])
    hy_f_w3 = din("hy_f_w3", [1, 64, 64])
    hy_f_b3 = din("hy_f_b3", [1, 64, 1])
    hy_f_freq = din("hy_f_freq", [1, 64, 1])
    hy_f_w4 = din("hy_f_w4", [1, 64, 4096])
    hy_skip = din("hy_skip", [1, 2, D])
    hy_w_out = din("hy_w_out", [1, D, D])
    hy_b_out = din("hy_b_out", [1, D])
    ffn_w_gate = din("ffn_w_gate", [2, D, DFF])
    ffn_w_up = din("ffn_w_up", [2, D, DFF])
    ffn_w_down = din("ffn_w_down", [2, DFF, D])
    moe_router = din("moe_router", [2, D, NE])
    moe_w_gate = din("moe_w_gate", [2, NE, D, DFE])
    moe_w_up = din("moe_w_up", [2, NE, D, DFE])
    moe_w_down = din("moe_w_down", [2, NE, DFE, D])
    ident_in = din("ident", [128, 128])
    identb_in = din("identb", [128, 128], BF16)
    maskb_in = din("maskb", [128, 384])
    ropec_in = din("ropec", [64, T])
    ropes_in = din("ropes", [64, T])
    ones_in = din("ones", [128, 128])
    absd_in = din("absd", [D])
    zT256_in = din("zT256", [33, 256])
    zT4096_in = din("zT4096", [33, 4096])
    negt256_in = din("negt256", [128, 2])
    negt4096_in = din("negt4096", [128, 32])
    FC256_in = din("FC256", [3, 128, 2, 128], BF16)
    FS256_in = din("FS256", [3, 128, 2, 128], BF16)
    GC256_in = din("GC256", [2, 128, 3, 128], BF16)
    GS256_in = din("GS256", [2, 128, 3, 128], BF16)
    FC4096_in = din("FC4096", [33, 128, 32, 128], BF16)
    FS4096_in = din("FS4096", [33, 128, 32, 128], BF16)
    GC4096_in = din("GC4096", [32, 128, 33, 128], BF16)
    GS4096_in = din("GS4096", [32, 128, 33, 128], BF16)
    out = nc.dram_tensor("out", [SEQ, D], F32, kind="ExternalOutput").ap()
    xs_out = nc.dram_tensor("xs_out", [T, D], F32, kind="ExternalOutput").ap()

    XS = dscr("XS", [T, D])
    MODS = dscr("MODS", [4, 2, 6 * D])
    if "mix" in parts:
        H2T = nc.dram_tensor("h2t_out", [8, 128, T], BF16, kind="ExternalOutput").ap()
        GATES = nc.dram_tensor("gts_out", [128, NBLK * NE], F32, kind="ExternalOutput").ap()
    elif "ffn" in parts:
        H2T = din("h2t_in", [8, 128, T], BF16)
        GATES = din("gts_in", [128, NBLK * NE])
    else:
        H2T = dscr("H2T", [8, 128, T], BF16)
        GATES = dscr("GATES", [128, NBLK * NE])
    gdB = Buf("gatesdram")
    OT = dscr("OT", [8, 128, T], BF16)
    GEL = dscr("GEL", [8, 128, T], BF16)
    UPRE = dscr("UPRE", [8, 128, T])
    xsbuf = [Buf(f"xs{i}") for i in range(NBLK)]
    otbuf = [Buf(f"ot{i}") for i in range(len(TILES))]
    h2buf = [Buf(f"h2{i}") for i in range(len(TILES))]

    ps = []
    for i in range(8):
        t = es.enter_context(nc.psum_tensor(f"ps{i}", [128, 512], F32))
        ps.append((t, Buf(f"ps{i}", psum=True)))
    psi = [0]

    def nps():
        psi[0] = (psi[0] + 1) % 8
        return ps[psi[0]]

    G = Phase(S)
    ident, identB = G.sb([128, 128], F32, "ident")
    identb, identbB = G.sb([128, 128], BF16, "identb")
    cds = S.dsem("const")
    S.dma("sp", ident[:], ident_in, cds, writes=[identB])
    S.dma("sp", identb[:], identb_in, cds, writes=[identbB])
    gates, gatesB = G.sb([128, NBLK, NE], F32, "gates")

    with Phase(S) as P:
        xcp = S.dsem("xcp")
        S.dma("sp", XS[:, :], xs_in, xcp, writes=xsbuf)
        c2, c2B = P.sb([128, 8, 2], F32, "c2")
        s2, s2B = P.sb([128, 8, 2], F32, "s2")
        S.dma("sp", c2[:], c2T, cds, writes=[c2B])
        S.op("act", lambda e: e.activation(out=s2[:], in_=c2[:], func=AF.Silu), reads=[c2B], writes=[s2B])
        wsl = [P.sb([128, 8, 512], F32, f"adaw{i}") + (S.dsem(f"adaw{i}"),) for i in range(2)]
        mrow, mrowB = P.sb([2, 6 * D], F32, "mrow")
        brow, browB = P.sb([2, 6 * D], F32, "brow")
        grow, growB = P.sb([2, 2 * D], F32, "grow")
        mds = S.dsem("mods")
        k = 0
        for L in layers:
            S.dma("sp", brow[:], ada_b[L, :].partition_broadcast(2), mds, writes=[browB])
            S.dma("sp", grow[:, 0:D], norm_mix[L, :].partition_broadcast(2), mds, writes=[growB])
            S.dma("sp", grow[:, D:2 * D], norm_ffn[L, :].partition_broadcast(2), mds, writes=[growB])
            for cg in range(12):
                w, wB, wd = wsl[k % 2]
                k += 1
                S.dma("sp", w[:], ada_w[L, :, cg * 512:(cg + 1) * 512].rearrange("(kc p) n -> p kc n", p=128), wd, writes=[wB])
                pt, pB = nps()
                for kc in range(8):
                    S.op("pe", lambda e, kc=kc, pt=pt, w=w: e.matmul(pt[0:2, :], s2[:, kc, :], w[:, kc, :], start=(kc == 0), stop=(kc == 7)),
                         reads=[s2B, wB], writes=[pB], inc=(kc == 7))
                S.op("dve", lambda e, pt=pt, cg=cg: e.tensor_tensor(out=mrow[:, cg * 512:(cg + 1) * 512], in0=pt[0:2, :], in1=brow[:, cg * 512:(cg + 1) * 512], op=ALU.add),
                     reads=[pB, browB], writes=[mrowB])
            for ch, go in ((1, 0), (4, D)):
                S.op("dve", lambda e, ch=ch, go=go: e.scalar_tensor_tensor(out=mrow[:, ch * D:(ch + 1) * D], in0=mrow[:, ch * D:(ch + 1) * D], scalar=1.0,
                                                                         in1=grow[:, go:go + D], op0=ALU.add, op1=ALU.mult),
                     reads=[mrowB, growB], writes=[mrowB])
            S.dma("sp", MODS[L, :, :], mrow[:], mds, reads=[mrowB])

    def load_mods(P, L, which, ks, ds):
        res = {}
        for kk in ks:
            t, tB = P.sb([128, D], F32, f"mod{L}_{which}_{kk}")
            S.dma("sp", t[:], MODS[L, which, kk * D:(kk + 1) * D].partition_broadcast(128), ds, writes=[tB])
            res[kk] = (t, tB)
        return res

    def rms_rstd(P, xt, xB, junk, junkB, st):
        stt, stB = st
        S.op("act", lambda e: e.activation(out=junk[:], in_=xt[:], func=AF.Square, accum_out=stt[:, 0:1]), reads=[xB], writes=[junkB, stB])
        S.op("act", lambda e: e.activation(out=stt[:, 1:2], in_=stt[:, 0:1], func=AF.Sqrt, scale=1.0 / D, bias=EPS), reads=[stB], writes=[stB])
        S.op("dve", lambda e: e.reciprocal(out=stt[:, 2:3], in_=stt[:, 1:2]), reads=[stB], writes=[stB])

    def norm_to_hT(xt, xB, tmp, tmpB, junk, junkB, st, A, sh, hT, hTB, col0, h32=None):
        rms_rstd(None, xt, xB, junk, junkB, st)
        stt, stB = st
        S.op("dve", lambda e: e.scalar_tensor_tensor(out=tmp[:], in0=xt[:], scalar=stt[:, 2:3], in1=A[0][:], op0=ALU.mult, op1=ALU.mult),
             reads=[xB, stB, A[1]], writes=[tmpB])
        S.op("dve", lambda e: e.tensor_tensor(out=tmp[:], in0=tmp[:], in1=sh[0][:], op=ALU.add), reads=[tmpB, sh[1]], writes=[tmpB])
        for half in range(2):
            pt, pB = nps()
            for q in range(4):
                kc = half * 4 + q
                S.op("pe", lambda e, pt=pt, q=q, kc=kc: e.transpose(pt[:, q * 128:(q + 1) * 128], tmp[:, kc * 128:(kc + 1) * 128], ident[:]),
                     reads=[tmpB, identB], writes=[pB], inc=(q == 3))
            S.op("act", lambda e, pt=pt, half=half: e.copy(out=hT[:, half * 4:half * 4 + 4, col0:col0 + 128], in_=pt[:, :].rearrange("p (a b) -> p a b", a=4)),
                 reads=[pB], writes=[hTB])
            if h32 is not None:
                S.op("dve", lambda e, pt=pt, half=half: e.tensor_copy(out=h32[0][:, half * 4:half * 4 + 4, :], in_=pt[:, :].rearrange("p (a b) -> p a b", a=4)),
                     reads=[pB], writes=[h32[1], pB])

    def phase_norm_proj(L, proj_setup, proj_tile, last):
        with Phase(S) as P:
            mds = S.dsem(f"m1_{L}")
            modl = load_mods(P, L, 0, (0, 1), mds)
            modc = load_mods(P, L, 1, (0, 1), mds)
            ctxo = proj_setup(P)
            xsl = [P.sb([128, D], F32, f"x{i}") + (S.dsem(f"x1_{L}_{i}"),) for i in range(3)]
            tmp, tmpB = P.sb([128, D], F32, "tmp")
            junk, junkB = P.sb([128, D], BF16, "junk")
            st = P.sb([128, 4], F32, "st")
            hTs = [P.sb([128, 8, 512], BF16, f"hT{i}") for i in range(2)]
            issued = set()

            def ensure_x(blk):
                if blk < NBLK and blk not in issued:
                    issued.add(blk)
                    xt, xB, xd = xsl[blk % 3]
                    S.dma("sp", xt[:], XS[blk * 128:(blk + 1) * 128, :], xd, reads=[xsbuf[blk]], writes=[xB])
            for ti, (t0, ntok) in enumerate(TILES):
                hT, hTB = hTs[ti % 2]
                md = modc if ti == 0 else modl
                for b in range(ntok // 128):
                    blk = t0 // 128 + b
                    ensure_x(blk)
                    ensure_x(blk + 1)
                    xt, xB, xd = xsl[blk % 3]
                    norm_to_hT(xt, xB, tmp, tmpB, junk, junkB, st, md[1], md[0], hT, hTB, b * 128)
                proj_tile(P, ctxo, ti, t0, ntok, hT, hTB)

    def phase_outproj_norm2(L, w_out_ap, bias_ap, last, moe_idx):
        if flags.get("no_moe_all"):
            moe_idx = None
        with Phase(S) as P:
            mds = S.dsem(f"m3_{L}")
            modl = load_mods(P, L, 0, (2, 3, 4), mds)
            modc = load_mods(P, L, 1, (2, 3, 4), mds)
            wo, woB = P.sb([128, 8, D], BF16, "wo")
            S.dma("pool", wo[:], w_out_ap.rearrange("(kc p) n -> p kc n", p=128), mds, writes=[woB])
            bo = None
            if bias_ap is not None:
                bo = P.sb([128, D], F32, "bo")
                S.dma("sp", bo[0][:], bias_ap.partition_broadcast(128), mds, writes=[bo[1]])
            if moe_idx is not None:
                rt, rtB = P.sb([128, 8, NE], F32, "rt")
                S.dma("sp", rt[:], moe_router[moe_idx].rearrange("(kc p) n -> p kc n", p=128), mds, writes=[rtB])
                h32 = P.sb([128, 8, 128], F32, "h32")
                lg, lgB = P.sb([128, NE], F32, "lg")
                m8, m8B = P.sb([128, 8], F32, "m8")
                wv, wvB = P.sb([128, 4], F32, "wv")
                e1, e1B = P.sb([128, NE], F32, "e1")
            ots = [P.sb([128, 8, 512], BF16, f"ot{i}") + (S.dsem(f"ot3_{L}_{i}"),) for i in range(2)]
            xsl = [P.sb([128, D], F32, f"x{i}") + (S.dsem(f"x3_{L}_{i}"),) for i in range(3)]
            tmp, tmpB = P.sb([128, D], F32, "tmp")
            tmp2, tmp2B = P.sb([128, D], F32, "tmp2")
            junk, junkB = P.sb([128, D], BF16, "junk")
            st = P.sb([128, 4], F32, "st")
            hTs = [P.sb([128, 8, 512], BF16, f"hT{i}") + (S.dsem(f"h2s_{L}_{i}"),) for i in range(2)]
            issued = set()

            def ensure_x(blk):
                if blk < NBLK and blk not in issued:
                    issued.add(blk)
                    xt, xB, xd = xsl[blk % 3]
                    S.dma("sp", xt[:], XS[blk * 128:(blk + 1) * 128, :], xd, reads=[xsbuf[blk]], writes=[xB])

            def ensure_ot(ti):
                if ti < len(TILES) and ("ot", ti) not in issued:
                    issued.add(("ot", ti))
                    t0_, ntok_ = TILES[ti]
                    ot, otB, otd = ots[ti % 2]
                    S.dma("sp", ot[:, :, 0:ntok_], OT[:, :, t0_:t0_ + ntok_].rearrange("j p t -> p j t"), otd, reads=[otbuf[ti]], writes=[otB])
            for ti, (t0, ntok) in enumerate(TILES):
                if last and ti == 0:
                    continue
                ensure_ot(ti)
                ensure_ot(ti + 1)
                ot, otB, otd = ots[ti % 2]
                hT, hTB, hTd = hTs[ti % 2]
                md = modc if ti == 0 else modl
                for b in range(ntok // 128):
                    blk = t0 // 128 + b
                    ensure_x(blk)
                    ensure_x(blk + 1)
                    xt, xB, xd = xsl[blk % 3]
                    for half in range(2):
                        pt, pB = nps()
                        for kc in range(8):
                            S.op("pe", lambda e, pt=pt, kc=kc, half=half, ot=ot, b=b: e.matmul(pt[:, :], ot[:, kc, b * 128:(b + 1) * 128], wo[:, kc, half * 512:(half + 1) * 512],
                                                                                          start=(kc == 0), stop=(kc == 7)),
                                 reads=[otB, woB], writes=[pB], inc=(kc == 7))
                        hs = slice(half * 512, (half + 1) * 512)
                        if bo is not None:
                            S.op("dve", lambda e, pt=pt, hs=hs: e.tensor_tensor(out=tmp2[:, hs], in0=pt[:, :], in1=bo[0][:, hs], op=ALU.add), reads=[pB, bo[1]], writes=[tmp2B])
                            S.op("dve", lambda e, hs=hs, md=md: e.tensor_tensor(out=tmp2[:, hs], in0=tmp2[:, hs], in1=md[2][0][:, hs], op=ALU.mult), reads=[tmp2B, md[2][1]], writes=[tmp2B])
                        else:
                            S.op("dve", lambda e, pt=pt, hs=hs, md=md: e.tensor_tensor(out=tmp2[:, hs], in0=pt[:, :], in1=md[2][0][:, hs], op=ALU.mult), reads=[pB, md[2][1]], writes=[tmp2B])
                    S.op("dve", lambda e, xt=xt: e.tensor_tensor(out=xt[:], in0=xt[:], in1=tmp2[:], op=ALU.add), reads=[xB, tmp2B], writes=[xB])
                    S.dma("sp", XS[blk * 128:(blk + 1) * 128, :], xt[:], xd, reads=[xB], writes=[xsbuf[blk]])
                    norm_to_hT(xt, xB, tmp, tmpB, junk, junkB, st, md[4], md[3], hT, hTB, b * 128, h32=(h32 if moe_idx is not None else None))
                    if moe_idx is not None and not flags.get("no_gate"):
                        pt, pB = nps()
                        for kc in range(8):
                            S.op("pe", lambda e, pt=pt, kc=kc: e.matmul(pt[:, 0:NE], h32[0][:, kc, :], rt[:, kc, :], start=(kc == 0), stop=(kc == 7)),
                                 reads=[h32[1], rtB], writes=[pB], inc=(kc == 7))
                        S.op("act", lambda e, pt=pt: e.copy(out=lg[:], in_=pt[:, 0:NE]), reads=[pB], writes=[lgB])
                        S.op("dve", lambda e: e.max(out=m8[:], in_=lg[:]), reads=[lgB], writes=[m8B])
                        S.op("dve", lambda e: e.tensor_tensor(out=wv[:, 0:1], in0=m8[:, 0:1], in1=m8[:, 1:2], op=ALU.subtract), reads=[m8B], writes=[wvB])
                        S.op("act", lambda e: e.activation(out=wv[:, 1:2], in_=wv[:, 0:1], func=AF.Sigmoid), reads=[wvB], writes=[wvB])
                        S.op("act", lambda e: e.activation(out=wv[:, 2:3], in_=wv[:, 0:1], func=AF.Sigmoid, scale=-1.0), reads=[wvB], writes=[wvB])
                        S.op("dve", lambda e: e.tensor_scalar(out=e1[:], in0=lg[:], scalar1=m8[:, 0:1], scalar2=wv[:, 1:2], op0=ALU.is_equal, op1=ALU.mult),
                             reads=[lgB, m8B, wvB], writes=[e1B])
                        S.op("dve", lambda e: e.tensor_scalar(out=lg[:], in0=lg[:], scalar1=m8[:, 1:2], scalar2=wv[:, 2:3], op0=ALU.is_equal, op1=ALU.mult),
                             reads=[lgB, m8B, wvB], writes=[lgB])
                        S.op("dve", lambda e, blk=blk: e.tensor_tensor(out=gates[:, blk, :], in0=e1[:], in1=lg[:], op=ALU.add), reads=[e1B, lgB], writes=[gatesB])
                S.dma("sp", H2T[:, :, t0:t0 + ntok].rearrange("j p t -> p j t"), hT[:, :, 0:ntok], hTd, reads=[hTB], writes=[h2buf[ti]])
            if moe_idx is not None and not flags.get("no_gstore"):
                S.dma("sp", GATES, gates[:].rearrange("p a b -> p (a b)"), mds, reads=[gatesB], writes=[gdB])

    def phase_ffn(L, dense_idx, moe_idx, last):
        if moe_idx is None:
            experts = [(ffn_w_gate[dense_idx], ffn_w_up[dense_idx], ffn_w_down[dense_idx])]
            dff = DFF
        else:
            experts = [(moe_w_gate[moe_idx, e], moe_w_up[moe_idx, e], moe_w_down[moe_idx, e]) for e in range(flags.get('moe_ne', NE))]
            dff = DFE
        groups = [(f0, min(512, dff - f0)) for f0 in range(0, dff, 512)]
        if last:
            supers = [[1, 2, 3, 4], [5, 6, 7, 8]]
        else:
            supers = [[0, 1, 2, 3, 4], [5, 6, 7, 8]]
        with Phase(S) as P:
            mds = S.dsem(f"m4_{L}")
            gl = load_mods(P, L, 0, (5,), mds)[5]
            if moe_idx is not None:
                S.dma("sp", gates[:].rearrange("p a b -> p (a b)"), GATES, mds, reads=[gdB], writes=[gatesB])
            gc = load_mods(P, L, 1, (5,), mds)[5]
            hT, hTB = P.sb([128, 8, 2304], BF16, "hTs")
            hd = S.dsem(f"h4_{L}")
            acc, accB = P.sb([128, 18, D], F32, "acc")
            wsl = []
            for i in range(2):
                wg, wgB = P.sb([128, 8, 512], BF16, f"wg{i}")
                wu, wuB = P.sb([128, 8, 512], BF16, f"wu{i}")
                wd, wdB = P.sb([128, 4, D], BF16, f"wd{i}")
                wsl.append((wg, wgB, wu, wuB, wd, wdB, S.dsem(f"w4_{L}_{i}")))
            acts = [P.sb([128, 4, 512], BF16, f"act{i}") for i in range(2)]
            sg = [P.sb([128, 512], F32, f"sg{i}") for i in range(2)]
            xsl = [P.sb([128, D], F32, f"x{i}") + (S.dsem(f"x4_{L}_{i}"),) for i in range(2)]
            tmp, tmpB = P.sb([128, D], F32, "tmp")
            wi = 0
            ai = 0
            xi = 0
            for sup in supers:
                col = 0
                cols = []
                for ti in sup:
                    t0, ntok = TILES[ti]
                    S.dma("sp", hT[:, :, col:col + ntok], H2T[:, :, t0:t0 + ntok].rearrange("j p t -> p j t"), hd, reads=[h2buf[ti]], writes=[hTB])
                    cols.append((col, ntok, t0))
                    col += ntok
                nblk = col // 128
                first = True
                for ei, (wg_ap, wu_ap, wd_ap) in enumerate(experts):
                    for gi, (f0, fw) in enumerate(groups):
                        nfc = fw // 128
                        wg, wgB, wu, wuB, wd, wdB, wds = wsl[wi % 2]
                        wi += 1
                        S.dma("pool", wg[:, :, 0:fw], wg_ap[:, f0:f0 + fw].rearrange("(kc p) n -> p kc n", p=128), wds, writes=[wgB])
                        S.dma("pool", wu[:, :, 0:fw], wu_ap[:, f0:f0 + fw].rearrange("(kc p) n -> p kc n", p=128), wds, writes=[wuB])
                        S.dma("pool", wd[:, 0:nfc, :], wd_ap[f0:f0 + fw, :].rearrange("(fc p) n -> p fc n", p=128), wds, writes=[wdB])
                        for (c0, ntok, t0) in cols:
                            act, actB = acts[ai % 2]
                            ai += 1
                            for fc in range(nfc):
                                pg, pgB = nps()
                                pu, puB = nps()
                                for kc in range(8):
                                    S.op("pe", lambda e, pg=pg, kc=kc, fc=fc, wg=wg, c0=c0, ntok=ntok: e.matmul(pg[:, 0:ntok], wg[:, kc, fc * 128:(fc + 1) * 128], hT[:, kc, c0:c0 + ntok],
                                                                                                             start=(kc == 0), stop=(kc == 7)),
                                         reads=[wgB, hTB], writes=[pgB], inc=(kc == 7))
                                for kc in range(8):
                                    S.op("pe", lambda e, pu=pu, kc=kc, fc=fc, wu=wu, c0=c0, ntok=ntok: e.matmul(pu[:, 0:ntok], wu[:, kc, fc * 128:(fc + 1) * 128], hT[:, kc, c0:c0 + ntok],
                                                                                                             start=(kc == 0), stop=(kc == 7)),
                                         reads=[wuB, hTB], writes=[puB], inc=(kc == 7))
                                sgt, sgB = sg[fc % 2]
                                S.op("act", lambda e, pg=pg, sgt=sgt, ntok=ntok: e.activation(out=sgt[:, 0:ntok], in_=pg[:, 0:ntok], func=AF.Silu), reads=[pgB], writes=[sgB])
                                S.op("dve", lambda e, pu=pu, sgt=sgt, act=act, fc=fc, ntok=ntok: e.tensor_tensor(out=act[:, fc, 0:ntok], in0=pu[:, 0:ntok], in1=sgt[:, 0:ntok], op=ALU.mult),
                                     reads=[puB, sgB], writes=[actB])
                            for b in range(ntok // 128):
                                ab = (c0 // 128) + b
                                blk = t0 // 128 + b
                                for half in range(2):
                                    pd, pdB = nps()
                                    for fc in range(nfc):
                                        S.op("pe", lambda e, pd=pd, fc=fc, act=act, b=b, wd=wd, half=half: e.matmul(pd[:, :], act[:, fc, b * 128:(b + 1) * 128], wd[:, fc, half * 512:(half + 1) * 512],
                                                                                                                 start=(fc == 0), stop=(fc == nfc - 1)),
                                             reads=[actB, wdB], writes=[pdB], inc=(fc == nfc - 1))
                                    hs = slice(half * 512, (half + 1) * 512)
                                    if moe_idx is None:
                                        if first:
                                            S.op("dve", lambda e, pd=pd, ab=ab, hs=hs: e.tensor_copy(out=acc[:, ab, hs], in_=pd[:, :]), reads=[pdB], writes=[accB])
                                        else:
                                            S.op("dve", lambda e, pd=pd, ab=ab, hs=hs: e.tensor_tensor(out=acc[:, ab, hs], in0=pd[:, :], in1=acc[:, ab, hs], op=ALU.add), reads=[pdB, accB], writes=[accB])
                                    else:
                                        if first:
                                            S.op("dve", lambda e, pd=pd, ab=ab, hs=hs, blk=blk, ei=ei: e.tensor_scalar(out=acc[:, ab, hs], in0=pd[:, :], scalar1=gates[:, blk, ei:ei + 1], scalar2=None, op0=ALU.mult),
                                                 reads=[pdB, gatesB], writes=[accB])
                                        else:
                                            S.op("dve", lambda e, pd=pd, ab=ab, hs=hs, blk=blk, ei=ei: e.scalar_tensor_tensor(out=acc[:, ab, hs], in0=pd[:, :], scalar=gates[:, blk, ei:ei + 1], in1=acc[:, ab, hs],
                                                                                                                         op0=ALU.mult, op1=ALU.add),
                                                 reads=[pdB, gatesB, accB], writes=[accB])
                        first = False
                for (c0, ntok, t0) in cols:
                    gm = gc if t0 == 0 else gl
                    for b in range(ntok // 128):
                        ab = (c0 // 128) + b
                        blk = t0 // 128 + b
                        xt, xB, xd = xsl[xi % 2]
                        xi += 1
                        S.dma("sp", xt[:], XS[blk * 128:(blk + 1) * 128, :], xd, reads=[xsbuf[blk]], writes=[xB])
                        S.op("dve", lambda e, ab=ab, gm=gm: e.tensor_tensor(out=tmp[:], in0=acc[:, ab, :], in1=gm[0][:], op=ALU.mult), reads=[accB, gm[1]], writes=[tmpB])
                        S.op("dve", lambda e, xt=xt: e.tensor_tensor(out=xt[:], in0=xt[:], in1=tmp[:], op=ALU.add), reads=[xB, tmpB], writes=[xB])
                        S.dma("sp", XS[blk * 128:(blk + 1) * 128, :], xt[:], xd, reads=[xB], writes=[xsbuf[blk]])

    def mixer_rglru(L, j, last):
        def setup(P):
            w, wB = P.sb([128, 8, 2 * D], BF16, "lruwin")
            d = S.dsem(f"lw_{L}")
            for h in range(2):
                S.dma("pool", w[:, :, h * D:(h + 1) * D], lru_w_in[j, :, h * D:(h + 1) * D].rearrange("(kc p) n -> p kc n", p=128), d, writes=[wB])
            gs = [P.sb([128, 8, 512], BF16, f"gst{i}") + (S.dsem(f"gst_{L}_{i}"),) for i in range(2)]
            us = [P.sb([128, 8, 512], F32, f"ust{i}") + (S.dsem(f"ust_{L}_{i}"),) for i in range(2)]
            return dict(w=w, wB=wB, gs=gs, us=us)

        def tile(P, c, ti, t0, ntok, hT, hTB):
            g, gB, gd = c["gs"][ti % 2]
            u, uB, ud = c["us"][ti % 2]
            w, wB = c["w"], c["wB"]
            for jj in range(8):
                pg, pgB = nps()
                for kc in range(8):
                    S.op("pe", lambda e, pg=pg, kc=kc, jj=jj: e.matmul(pg[:, 0:ntok], w[:, kc, jj * 128:(jj + 1) * 128], hT[:, kc, 0:ntok], start=(kc == 0), stop=(kc == 7)),
                         reads=[wB, hTB], writes=[pgB], inc=(kc == 7))
                S.op("act", lambda e, pg=pg, jj=jj: e.activation(out=g[:, jj, 0:ntok], in_=pg[:, 0:ntok], func=AF.Gelu_apprx_tanh), reads=[pgB], writes=[gB])
                pu, puB = nps()
                for kc in range(8):
                    S.op("pe", lambda e, pu=pu, kc=kc, jj=jj: e.matmul(pu[:, 0:ntok], w[:, kc, D + jj * 128:D + (jj + 1) * 128], hT[:, kc, 0:ntok], start=(kc == 0), stop=(kc == 7)),
                         reads=[wB, hTB], writes=[puB], inc=(kc == 7))
                S.op("dve", lambda e, pu=pu, jj=jj: e.tensor_copy(out=u[:, jj, 0:ntok], in_=pu[:, 0:ntok]), reads=[puB], writes=[uB])
            S.dma("sp", GEL[:, :, t0:t0 + ntok].rearrange("j p t -> p j t"), g[:, :, 0:ntok], gd, reads=[gB])
            S.dma("sp", UPRE[:, :, t0:t0 + ntok].rearrange("j p t -> p j t"), u[:, :, 0:ntok], ud, reads=[uB])

        phase_norm_proj(L, setup, tile, last)

        with Phase(S) as P:
            cd = S.dsem(f"lc_{L}")
            cw, cwB = P.sb([128, 8, 4], F32, "cw")
            cb, cbB = P.sb([128, 8], F32, "cb")
            ba, baB = P.sb([128, 2, 8], F32, "ba")
            bx, bxB = P.sb([128, 2, 8], F32, "bx")
            lam, lamB = P.sb([128, 2, 8], F32, "lam")
            cdv, cdvB = P.sb([128, 2, 8], F32, "cdv")
            cdv2, cdv2B = P.sb([128, 2, 8], F32, "cdv2")
            S.dma("sp", cw[:], lru_conv_wT[j], cd, writes=[cwB])
            S.dma("sp", cb[:], lru_conv_bT[j], cd, writes=[cbB])
            for d in range(2):
                S.dma("sp", ba[:, d, :], lru_b_aT[j, d], cd, writes=[baB])
                S.dma("sp", bx[:, d, :], lru_b_xT[j, d], cd, writes=[bxB])
                S.dma("sp", lam[:, d, :], lru_lamT[j, d], cd, writes=[lamB])
            S.op("act", lambda e: e.activation(out=cdv[:], in_=lam[:], func=AF.Exp, scale=-1.0), reads=[lamB], writes=[cdvB])
            S.op("act", lambda e: e.activation(out=cdv[:], in_=cdv[:], func=AF.Ln, bias=1.0), reads=[cdvB], writes=[cdvB])
            S.op("dve", lambda e: e.tensor_scalar(out=cdv2[:], in0=cdv[:], scalar1=-16.0, scalar2=None, op0=ALU.mult), reads=[cdvB], writes=[cdv2B])
            S.op("dve", lambda e: e.tensor_scalar(out=cdv[:], in0=cdv[:], scalar1=-8.0, scalar2=None, op0=ALU.mult), reads=[cdvB], writes=[cdvB])
            wa, waB = P.sb([128, 2, 8, 128], BF16, "wa")
            wx, wxB = P.sb([128, 2, 8, 128], BF16, "wx")
            for d in range(2):
                S.dma("pool", wa[:, d, :, :], lru_w_a[j, d].rearrange("h i o -> i h o"), cd, writes=[waB])
                S.dma("pool", wx[:, d, :, :], lru_w_x[j, d].rearrange("h i o -> i h o"), cd, writes=[wxB])
            UP, UPB = P.sb([128, T], F32, "UP")
            U, UB_ = P.sb([128, T], F32, "U")
            Ub, UbB = P.sb([128, T], BF16, "Ub")
            Rr, RB = P.sb([128, T], F32, "R")
            Ii, IB = P.sb([128, T], F32, "I")
            Aa, AB = P.sb([128, T], F32, "A")
            Bb, BB = P.sb([128, T], F32, "Bv")
            Y0, Y0B = P.sb([128, T], F32, "Y0")
            Y1, Y1B = P.sb([128, T], F32, "Y1")
            Gg, GB = P.sb([128, T], BF16, "Gg")
            Oo, OB = P.sb([128, T], BF16, "Oo")
            ld = S.dsem(f"ll_{L}")
            ld2 = S.dsem(f"ll2_{L}")
            segs = [(0, CTX), (CTX, T)]
            for jj in range(8):
                S.dma("sp", UP[:], UPRE[jj, :, :], ld, writes=[UPB])
                S.dma("sp", Gg[:], GEL[jj, :, :], ld2, writes=[GB])
                S.op("act", lambda e, jj=jj: e.activation(out=U[:], in_=UP[:], func=AF.Identity, scale=cw[:, jj, 2:3], bias=cb[:, jj:jj + 1]), reads=[UPB, cwB, cbB], writes=[UB_])
                for (a, b_) in segs:
                    for (tap, sh) in ((0, -2), (1, -1), (3, 1)):
                        lo = max(a, a - sh)
                        hi = min(b_, b_ - sh)
                        S.op("dve", lambda e, jj=jj, tap=tap, sh=sh, lo=lo, hi=hi: e.scalar_tensor_tensor(out=U[:, lo:hi], in0=UP[:, lo + sh:hi + sh], scalar=cw[:, jj, tap:tap + 1], in1=U[:, lo:hi],
                                                                                                   op0=ALU.mult, op1=ALU.add),
                             reads=[UPB, cwB, UB_], writes=[UB_])
                S.op("act", lambda e: e.copy(out=Ub[:], in_=U[:]), reads=[UB_], writes=[UbB])
                for d in range(2):
                    for (t0, ntok) in TILES:
                        pr, prB = nps()
                        S.op("pe", lambda e, pr=pr, d=d, jj=jj, t0=t0, ntok=ntok: e.matmul(pr[:, 0:ntok], wa[:, d, jj, :], Ub[:, t0:t0 + ntok], start=True, stop=True), reads=[waB, UbB], writes=[prB])
                        S.op("act", lambda e, pr=pr, d=d, jj=jj, t0=t0, ntok=ntok: e.activation(out=Rr[:, t0:t0 + ntok], in_=pr[:, 0:ntok], func=AF.Sigmoid, bias=ba[:, d, jj:jj + 1]), reads=[prB, baB], writes=[RB])
                        pi_, piB = nps()
                        S.op("pe", lambda e, pi_=pi_, d=d, jj=jj, t0=t0, ntok=ntok: e.matmul(pi_[:, 0:ntok], wx[:, d, jj, :], Ub[:, t0:t0 + ntok], start=True, stop=True), reads=[wxB, UbB], writes=[piB])
                        S.op("act", lambda e, pi_=pi_, d=d, jj=jj, t0=t0, ntok=ntok: e.activation(out=Ii[:, t0:t0 + ntok], in_=pi_[:, 0:ntok], func=AF.Sigmoid, bias=bx[:, d, jj:jj + 1]), reads=[piB, bxB], writes=[IB])
                    S.op("act", lambda e, d=d, jj=jj: e.activation(out=Aa[:], in_=Rr[:], func=AF.Exp, scale=cdv[:, d, jj:jj + 1]), reads=[RB, cdvB], writes=[AB])
                    S.op("act", lambda e, d=d, jj=jj: e.activation(out=Rr[:], in_=Rr[:], func=AF.Exp, scale=cdv2[:, d, jj:jj + 1]), reads=[RB, cdv2B], writes=[RB])
                    S.op("dve", lambda e: e.tensor_scalar(out=Rr[:], in0=Rr[:], scalar1=-1.0, scalar2=1.0, op0=ALU.mult, op1=ALU.add), reads=[RB], writes=[RB])
                    S.op("act", lambda e: e.activation(out=Rr[:], in_=Rr[:], func=AF.Sqrt), reads=[RB], writes=[RB])
                    S.op("dve", lambda e: e.tensor_tensor(out=Ii[:], in0=Ii[:], in1=U[:], op=ALU.mult), reads=[IB, UB_], writes=[IB])
                    S.op("dve", lambda e: e.tensor_tensor(out=Bb[:], in0=Rr[:], in1=Ii[:], op=ALU.mult), reads=[RB, IB], writes=[BB])
                    if d == 0:
                        S.op("dve", lambda e: e.tensor_tensor_scan(out=Y0[:], data0=Aa[:], data1=Bb[:], initial=0.0, op0=ALU.mult, op1=ALU.add), reads=[AB, BB], writes=[Y0B])
                    else:
                        S.op("dve", lambda e: e.tensor_tensor_scan(out=Y1[:, CTX - 1::-1], data0=Aa[:, CTX - 1::-1], data1=Bb[:, CTX - 1::-1], initial=0.0, op0=ALU.mult, op1=ALU.add),
                             reads=[AB, BB], writes=[Y1B])
                        S.op("dve", lambda e: e.tensor_tensor_scan(out=Y1[:, T - 1:CTX - 1:-1], data0=Aa[:, T - 1:CTX - 1:-1], data1=Bb[:, T - 1:CTX - 1:-1], initial=Y1[:, 0:1], op0=ALU.mult, op1=ALU.add),
                             reads=[AB, BB, Y1B], writes=[Y1B])
                S.op("dve", lambda e: e.tensor_tensor(out=Y0[:], in0=Y0[:], in1=Y1[:], op=ALU.add), reads=[Y0B, Y1B], writes=[Y0B])
                S.op("dve", lambda e: e.tensor_tensor(out=Oo[:], in0=Y0[:], in1=Gg[:], op=ALU.mult), reads=[Y0B, GB], writes=[OB])
                S.dma("sp", OT[jj, :, :], Oo[:], ld2, reads=[OB], writes=otbuf)
        phase_outproj_norm2(L, lru_w_out[j], None, last, (L // 2) if L % 2 == 1 else None)


    def mixer_swa(L, last):
        QT = dscr("QT", [16, 64, T], BF16)
        KT = dscr("KT", [4, 64, T], BF16)
        VV = dscr("VV", [T, 256], BF16)
        SC = 0.125

        def setup(P):
            d = S.dsem(f"aw_{L}")
            w, wB = P.sb([128, 8, 1536], BF16, "wqkv")
            wp, wpB = P.sb([128, 8, 1280], BF16, "wqkp")
            S.dma("pool", w[:], attn_w_qkv[0].rearrange("(kc p) n -> p kc n", p=128), d, writes=[wB])
            S.dma("pool", wp[:], attn_w_qkp[0].rearrange("(kc p) n -> p kc n", p=128), d, writes=[wpB])
            rcs = [P.sb([64, 2, 512], F32, f"rc{i}") + (S.dsem(f"rc_{L}_{i}"),) for i in range(2)]
            qst = [P.sb([64, 20, 512], BF16, f"qst{i}") + (S.dsem(f"qst_{L}_{i}"),) for i in range(2)]
            vst = [P.sb([128, 4, 256], BF16, f"vst{i}") + (S.dsem(f"vst_{L}_{i}"),) for i in range(2)]
            t1 = P.sb([64, 512], F32, "t1")
            t2 = P.sb([64, 512], F32, "t2")
            return dict(w=w, wB=wB, wp=wp, wpB=wpB, rcs=rcs, qst=qst, vst=vst, t1=t1, t2=t2)

        def tile(P, c, ti, t0, ntok, hT, hTB):
            w, wB, wp, wpB = c["w"], c["wB"], c["wp"], c["wpB"]
            rc, rcB, rcd = c["rcs"][ti % 2]
            q, qB, qd = c["qst"][ti % 2]
            v, vB, vd = c["vst"][ti % 2]
            t1, t1B = c["t1"]
            t2, t2B = c["t2"]
            S.dma("sp", rc[:, 0, 0:ntok], ropec_in[:, t0:t0 + ntok], rcd, writes=[rcB])
            S.dma("sp", rc[:, 1, 0:ntok], ropes_in[:, t0:t0 + ntok], rcd, writes=[rcB])
            for hh in range(20):
                pq, pqB = nps()
                for kc in range(8):
                    S.op("pe", lambda e: e.matmul(pq[0:64, 0:ntok], w[:, kc, hh * 64:(hh + 1) * 64], hT[:, kc, 0:ntok], start=(kc == 0), stop=(kc == 7)),
                         reads=[wB, hTB], writes=[pqB], inc=(kc == 7))
                pp, ppB = nps()
                for kc in range(8):
                    S.op("pe", lambda e: e.matmul(pp[0:64, 0:ntok], wp[:, kc, hh * 64:(hh + 1) * 64], hT[:, kc, 0:ntok], start=(kc == 0), stop=(kc == 7)),
                         reads=[wpB, hTB], writes=[ppB], inc=(kc == 7))
                S.op("dve", lambda e: e.tensor_tensor(out=t1[:, 0:ntok], in0=pq[0:64, 0:ntok], in1=rc[:, 0, 0:ntok], op=ALU.mult), reads=[pqB, rcB], writes=[t1B])
                S.op("dve", lambda e: e.tensor_tensor(out=t2[:, 0:ntok], in0=pp[0:64, 0:ntok], in1=rc[:, 1, 0:ntok], op=ALU.mult), reads=[ppB, rcB], writes=[t2B])
                S.op("dve", lambda e: e.tensor_tensor(out=q[:, hh, 0:ntok], in0=t1[:, 0:ntok], in1=t2[:, 0:ntok], op=ALU.add), reads=[t1B, t2B], writes=[qB])
            for b in range(ntok // 128):
                pv, pvB = nps()
                for kc in range(8):
                    S.op("pe", lambda e: e.matmul(pv[:, 0:256], hT[:, kc, b * 128:(b + 1) * 128], w[:, kc, 1280:1536], start=(kc == 0), stop=(kc == 7)),
                         reads=[wB, hTB], writes=[pvB], inc=(kc == 7))
                S.op("act", lambda e: e.copy(out=v[:, b, :], in_=pv[:, 0:256]), reads=[pvB], writes=[vB])
            S.dma("sp", QT[:, :, t0:t0 + ntok].rearrange("h d t -> d h t"), q[:, 0:16, 0:ntok], qd, reads=[qB])
            S.dma("sp", KT[:, :, t0:t0 + ntok].rearrange("h d t -> d h t"), q[:, 16:20, 0:ntok], qd, reads=[qB])
            S.dma("sp", VV[t0:t0 + ntok, :].rearrange("(b p) c -> p b c", p=128), v[:, 0:ntok // 128, :], vd, reads=[vB])

        phase_norm_proj(L, setup, tile, last)
        if flags.get("swa_p1only"):
            return

        with Phase(S) as P:
            d = S.dsem(f"ak_{L}")
            KTs, KTB = P.sb([64, 4, T], BF16, "KTs")
            Vs, VB = P.sb([128, NBLK, 256], BF16, "Vs")
            mb, mbB = P.sb([128, 384], F32, "mb")
            sk, skB = P.sb([128, 16], F32, "sk")
            nsk, nskB = P.sb([128, 16], F32, "nsk")
            S.dma("sp", KTs[:], KT.rearrange("h d t -> d h t"), d, writes=[KTB])
            S.dma("sp", Vs[:], VV.rearrange("(b p) c -> p b c", p=128), d, writes=[VB])
            S.dma("sp", mb[:], maskb_in, d, writes=[mbB])
            S.dma("sp", sk[:], attn_sinks[0, :].partition_broadcast(128), d, writes=[skB])
            S.op("dve", lambda e: e.tensor_scalar(out=nsk[:], in0=sk[:], scalar1=-1.0, scalar2=None, op0=ALU.mult), reads=[skB], writes=[nskB])
            qts = [P.sb([64, 16, 128], BF16, f"qt{i}") + (S.dsem(f"qt_{L}_{i}"),) for i in range(2)]
            oto = [P.sb([128, D], BF16, f"oto{i}") for i in range(2)]
            ost = [P.sb([128, 8, 128], BF16, f"ost{i}") + (S.dsem(f"ost_{L}_{i}"),) for i in range(2)]
            Ssb = [P.sb([128, 640], F32, f"Ssb{i}") for i in range(2)]
            Pbs = [P.sb([128, 640], BF16, f"Pb{i}") for i in range(2)]
            PTs = [P.sb([128, 5, 128], BF16, f"PT{i}") for i in range(2)]
            sts = [P.sb([128, 8], F32, f"ast{i}") for i in range(2)]
            NQB = flags.get("swa_nqb", NBLK)
            items = [(qb, h) for qb in range(NQB) for h in range(16)]
            ctxs = {}

            def qb_info(qb):
                if qb < 2:
                    lks, m0 = [], 0
                else:
                    lq = qb - 2
                    lks = [x for x in (lq - 1, lq, lq + 1) if 0 <= x < 32]
                    m0 = 0 if lq - 1 >= 0 else 128
                nloc = len(lks) * 128
                return lks, m0, nloc, nloc + 256, [2 + x for x in lks] + [0, 1]

            def stage_a(i):
                qb, h = items[i]
                qt, qtB, qtd = qts[qb % 2]
                if h == 0:
                    S.dma("sp", qt[:], QT[:, :, qb * 128:(qb + 1) * 128].rearrange("h d t -> d h t"), qtd, writes=[qtB])
                lks, m0, nloc, n, vblks = qb_info(qb)
                kh = h // 4
                Sb, SbB = Ssb[i % 2]
                Pb, PbB = Pbs[i % 2]
                st_, stB_ = sts[i % 2]
                if nloc:
                    pa, paB = nps()
                    k0 = CTX + lks[0] * 128
                    S.op("pe", lambda e: e.matmul(pa[:, 0:nloc], qt[:, h, :], KTs[:, kh, k0:k0 + nloc], start=True, stop=True), reads=[qtB, KTB], writes=[paB])
                    S.op("dve", lambda e: e.tensor_tensor(out=Sb[:, 0:nloc], in0=pa[:, 0:nloc], in1=mb[:, m0:m0 + nloc], op=ALU.add), reads=[paB, mbB], writes=[SbB])
                pc, pcB = nps()
                S.op("pe", lambda e: e.matmul(pc[:, 0:256], qt[:, h, :], KTs[:, kh, 0:256], start=True, stop=True), reads=[qtB, KTB], writes=[pcB])
                S.op("act", lambda e: e.copy(out=Sb[:, nloc:n], in_=pc[:, 0:256]), reads=[pcB], writes=[SbB])
                S.op("dve", lambda e: e.reduce_max(out=st_[:, 0:1], in_=Sb[:, 0:n], axis=AX.X), reads=[SbB], writes=[stB_])
                S.op("dve", lambda e: e.tensor_scalar(out=st_[:, 1:2], in0=st_[:, 0:1], scalar1=-SC, scalar2=nsk[:, h:h + 1], op0=ALU.mult, op1=ALU.min), reads=[stB_, nskB], writes=[stB_])
                S.op("act", lambda e: e.activation(out=Pb[:, 0:n], in_=Sb[:, 0:n], func=AF.Exp, scale=SC, bias=st_[:, 1:2], accum_out=st_[:, 2:3]), reads=[SbB, stB_], writes=[PbB, stB_])
                S.op("act", lambda e: e.activation(out=st_[:, 3:4], in_=st_[:, 1:2], func=AF.Exp, bias=sk[:, h:h + 1]), reads=[stB_, skB], writes=[stB_])
                S.op("dve", lambda e: e.tensor_tensor(out=st_[:, 4:5], in0=st_[:, 2:3], in1=st_[:, 3:4], op=ALU.add), reads=[stB_], writes=[stB_])
                S.op("dve", lambda e: e.reciprocal(out=st_[:, 5:6], in_=st_[:, 4:5]), reads=[stB_], writes=[stB_])

            def stage_b(i):
                qb, h = items[i]
                lks, m0, nloc, n, vblks = qb_info(qb)
                kh = h // 4
                Pb, PbB = Pbs[i % 2]
                PT, PTB = PTs[i % 2]
                st_, stB_ = sts[i % 2]
                ot_, otB_ = oto[qb % 2]
                nkb = n // 128
                pt, ptB = nps()
                ptb = pt[:, :].bitcast(BF16)
                for kb in range(nkb):
                    S.op("pe", lambda e: e.transpose(ptb[:, kb * 128:(kb + 1) * 128], Pb[:, kb * 128:(kb + 1) * 128], identb[:]), reads=[PbB, identbB], writes=[ptB], inc=(kb == nkb - 1))
                S.op("act", lambda e: e.copy(out=PT[:, 0:nkb, :], in_=ptb[:, 0:n].rearrange("p (a b) -> p a b", a=nkb)), reads=[ptB], writes=[PTB])
                po, poB = nps()
                for kb in range(nkb):
                    S.op("pe", lambda e: e.matmul(po[:, 0:64], PT[:, kb, :], Vs[:, vblks[kb], kh * 64:(kh + 1) * 64], start=(kb == 0), stop=(kb == nkb - 1)),
                         reads=[PTB, VB], writes=[poB], inc=(kb == nkb - 1))
                S.op("dve", lambda e: e.tensor_scalar(out=ot_[:, h * 64:(h + 1) * 64], in0=po[:, 0:64], scalar1=st_[:, 5:6], scalar2=None, op0=ALU.mult), reads=[poB, stB_], writes=[otB_])
                if h == 15:
                    os_, osB, osd = ost[qb % 2]
                    pt, ptB = nps()
                    ptb = pt[:, :].bitcast(BF16)
                    for kc in range(8):
                        S.op("pe", lambda e: e.transpose(ptb[:, kc * 128:(kc + 1) * 128], ot_[:, kc * 128:(kc + 1) * 128], identb[:]), reads=[otB_, identbB], writes=[ptB], inc=(kc == 7))
                    S.op("act", lambda e: e.copy(out=os_[:], in_=ptb[:, :].rearrange("p (a b) -> p a b", a=8)), reads=[ptB], writes=[osB])
                    S.dma("sp", OT[:, :, qb * 128:(qb + 1) * 128].rearrange("j p t -> p j t"), os_[:], osd, reads=[osB], writes=otbuf)

            for i in range(len(items) + 1):
                if i < len(items):
                    stage_a(i)
                if i >= 1:
                    stage_b(i - 1)
        if not flags.get('no_outproj'):
            phase_outproj_norm2(L, attn_w_o[0], None, last, L // 2)


    def mixer_hyena(L, last):
        UH = dscr("UH", [24, 128, T])
        UT = dscr("UT", [3, T, D])
        Z1 = dscr("Z1", [T, D])
        HID = {256: dscr("HID256", [64, 256]), 4096: dscr("HID4096", [64, 4096])}
        APM = {Ln: dscr(f"APM{Ln}", [2, 2, 128, Ln // 128, D], BF16) for Ln in (256, 4096)}
        RIN = {Ln: dscr(f"RIN{Ln}", [2, D]) for Ln in (256, 4096)}
        KF = {Ln: dscr(f"KF{Ln}", [2, 2, (Ln // 128 + 1) * 128, D]) for Ln in (256, 4096)}
        YS = {Ln: dscr(f"YS{Ln}", [2, (Ln // 128 + 1) * 128, D], BF16) for Ln in (256, 4096)}
        zT = {256: zT256_in, 4096: zT4096_in}
        negt = {256: negt256_in, 4096: negt4096_in}
        FCm = {256: (FC256_in, FS256_in), 4096: (FC4096_in, FS4096_in)}
        GCm = {256: (GC256_in, GS256_in), 4096: (GC4096_in, GS4096_in)}
        TOK0 = {256: 0, 4096: CTX}
        PI = 3.1415925

        def setup(P):
            d = S.dsem(f"hw_{L}")
            w, wB = P.sb([128, 8, 3 * D], BF16, "hywin")
            for h in range(3):
                S.dma("pool", w[:, :, h * D:(h + 1) * D], hy_w_in[0, :, h * D:(h + 1) * D].rearrange("(kc p) n -> p kc n", p=128), d, writes=[wB])
            bi, biB = P.sb([128, 24], F32, "hybin")
            S.dma("sp", bi[:], hy_b_inT[0], d, writes=[biB])
            us = [P.sb([128, 12, 512], F32, f"hust{i}") + (S.dsem(f"hust_{L}_{i}"),) for i in range(2)]
            return dict(w=w, wB=wB, bi=bi, biB=biB, us=us, k=[0])

        def tile(P, c, ti, t0, ntok, hT, hTB):
            w, wB, bi, biB = c["w"], c["wB"], c["bi"], c["biB"]
            for hf in range(2):
                u, uB, ud = c["us"][c["k"][0] % 2]
                c["k"][0] += 1
                for jj in range(12):
                    ch = hf * 12 + jj
                    pu, puB = nps()
                    for kc in range(8):
                        S.op("pe", lambda e: e.matmul(pu[:, 0:ntok], w[:, kc, ch * 128:(ch + 1) * 128], hT[:, kc, 0:ntok], start=(kc == 0), stop=(kc == 7)),
                             reads=[wB, hTB], writes=[puB], inc=(kc == 7))
                    S.op("act", lambda e: e.activation(out=u[:, jj, 0:ntok], in_=pu[:, 0:ntok], func=AF.Identity, bias=bi[:, ch:ch + 1]), reads=[puB, biB], writes=[uB])
                S.dma("sp", UH[hf * 12:(hf + 1) * 12, :, t0:t0 + ntok].rearrange("j p t -> p j t"), u[:, :, 0:ntok], ud, reads=[uB])

        phase_norm_proj(L, setup, tile, last)

        with Phase(S) as P:
            d = S.dsem(f"hc_{L}")
            cw, cwB = P.sb([128, 24, 3], F32, "hcw")
            cb, cbB = P.sb([128, 24], F32, "hcb")
            S.dma("sp", cw[:], hy_conv_wT[0], d, writes=[cwB])
            S.dma("sp", cb[:], hy_conv_bT[0], d, writes=[cbB])
            ups = [P.sb([128, T], F32, f"hup{i}") + (S.dsem(f"hup_{L}_{i}"),) for i in range(2)]
            U, UB_ = P.sb([128, T], F32, "hU")
            tos = [P.sb([128, NBLK, 128], F32, f"hto{i}") + (S.dsem(f"hto_{L}_{i}"),) for i in range(2)]
            segs = [(0, CTX), (CTX, T)]
            for ch in range(24):
                UP, UPB, upd = ups[ch % 2]
                to, toB, tod = tos[ch % 2]
                S.dma("sp", UP[:], UH[ch, :, :], upd, writes=[UPB])
                S.op("act", lambda e: e.activation(out=U[:], in_=UP[:], func=AF.Identity, scale=cw[:, ch, 1:2], bias=cb[:, ch:ch + 1]), reads=[UPB, cwB, cbB], writes=[UB_])
                for (a, b_) in segs:
                    for (tap, sh) in ((0, -1), (2, 1)):
                        lo = max(a, a - sh)
                        hi = min(b_, b_ - sh)
                        S.op("dve", lambda e: e.scalar_tensor_tensor(out=U[:, lo:hi], in0=UP[:, lo + sh:hi + sh], scalar=cw[:, ch, tap:tap + 1], in1=U[:, lo:hi], op0=ALU.mult, op1=ALU.add),
                             reads=[UPB, cwB, UB_], writes=[UB_])
                for g4 in range(0, NBLK, 4):
                    nb4 = min(4, NBLK - g4)
                    pt, pB = nps()
                    for q in range(nb4):
                        S.op("pe", lambda e: e.transpose(pt[:, q * 128:(q + 1) * 128], U[:, (g4 + q) * 128:(g4 + q + 1) * 128], ident[:]), reads=[UB_, identB], writes=[pB], inc=(q == nb4 - 1))
                    S.op("act", lambda e: e.copy(out=to[:, g4:g4 + nb4, :], in_=pt[:, 0:nb4 * 128].rearrange("p (a b) -> p a b", a=nb4)), reads=[pB], writes=[toB])
                which, cc = ch // 8, ch % 8
                S.dma("sp", UT[which, :, cc * 128:(cc + 1) * 128].rearrange("(b p) c -> p b c", p=128), to[:], tod, reads=[toB])

        def range_reduce(arg, argB, ki, kiB, tmp, tmpB, n):
            S.op("dve", lambda e: e.tensor_scalar(out=ki[:, 0:n], in0=arg[:, 0:n], scalar1=1.0 / (2 * math.pi), scalar2=64.5, op0=ALU.mult, op1=ALU.add), reads=[argB], writes=[kiB])
            S.op("dve", lambda e: e.tensor_copy(out=tmp[:, 0:n], in_=ki[:, 0:n]), reads=[kiB], writes=[tmpB])
            S.op("dve", lambda e: e.tensor_scalar(out=tmp[:, 0:n], in0=tmp[:, 0:n], scalar1=-64.0, scalar2=-2 * math.pi, op0=ALU.add, op1=ALU.mult), reads=[tmpB], writes=[tmpB])
            S.op("dve", lambda e: e.tensor_tensor(out=arg[:, 0:n], in0=arg[:, 0:n], in1=tmp[:, 0:n], op=ALU.add), reads=[argB, tmpB], writes=[argB])
            S.op("dve", lambda e: e.tensor_scalar(out=tmp[:, 0:n], in0=arg[:, 0:n], scalar1=-PI, scalar2=2 * math.pi, op0=ALU.is_lt, op1=ALU.mult), reads=[argB], writes=[tmpB])
            S.op("dve", lambda e: e.tensor_tensor(out=arg[:, 0:n], in0=arg[:, 0:n], in1=tmp[:, 0:n], op=ALU.add), reads=[argB, tmpB], writes=[argB])
            S.op("dve", lambda e: e.tensor_scalar(out=tmp[:, 0:n], in0=arg[:, 0:n], scalar1=PI, scalar2=-2 * math.pi, op0=ALU.is_gt, op1=ALU.mult), reads=[argB], writes=[tmpB])
            S.op("dve", lambda e: e.tensor_tensor(out=arg[:, 0:n], in0=arg[:, 0:n], in1=tmp[:, 0:n], op=ALU.add), reads=[argB, tmpB], writes=[argB])
            S.op("dve", lambda e: e.tensor_scalar(out=arg[:, 0:n], in0=arg[:, 0:n], scalar1=-PI, scalar2=PI, op0=ALU.max, op1=ALU.min), reads=[argB], writes=[argB])

        with Phase(S) as P:
            d = S.dsem(f"hf_{L}")
            w1, w1B = P.sb([33, 64], F32, "fw1")
            w2, w2B = P.sb([64, 64], F32, "fw2")
            w3, w3B = P.sb([64, 64], F32, "fw3")
            fb, fbB = P.sb([64, 4], F32, "ffb")
            S.dma("sp", w1[:], hy_f_w1[0], d, writes=[w1B])
            S.dma("sp", w2[:], hy_f_w2[0], d, writes=[w2B])
            S.dma("sp", w3[:], hy_f_w3[0], d, writes=[w3B])
            for i, ap in enumerate((hy_f_b1, hy_f_b2, hy_f_b3, hy_f_freq)):
                S.dma("sp", fb[:, i:i + 1], ap[0], d, writes=[fbB])
            S.op("dve", lambda e: e.tensor_scalar(out=fb[:, 0:3], in0=fb[:, 0:3], scalar1=fb[:, 3:4], scalar2=None, op0=ALU.mult), reads=[fbB], writes=[fbB])
            zt, ztB = P.sb([33, 4096], F32, "zt")
            ha, haB = P.sb([64, 4096], F32, "ha")
            hb, hbB = P.sb([64, 4096], F32, "hb")
            ki, kiB = P.sb([64, 4096], I32, "ki")
            tmp, tmpB = P.sb([64, 4096], F32, "ftmp")
            for Ln in (256, 4096):
                S.dma("sp", zt[:, 0:Ln], zT[Ln], d, writes=[ztB])
                src, srcB, K_ = zt, ztB, 33
                for li, (w_, wB_) in enumerate(((w1, w1B), (w2, w2B), (w3, w3B))):
                    dst, dstB = (ha, haB) if li % 2 == 0 else (hb, hbB)
                    for c0 in range(0, Ln, 512):
                        n = min(512, Ln - c0)
                        pp, ppB = nps()
                        S.op("pe", lambda e: e.matmul(pp[0:64, 0:n], w_[0:K_, :], src[0:K_, c0:c0 + n], start=True, stop=True), reads=[wB_, srcB], writes=[ppB])
                        S.op("act", lambda e: e.activation(out=dst[:, c0:c0 + n], in_=pp[0:64, 0:n], func=AF.Identity, scale=fb[:, 3:4], bias=fb[:, li:li + 1]), reads=[ppB, fbB], writes=[dstB])
                    range_reduce(dst, dstB, ki, kiB, tmp, tmpB, Ln)
                    S.op("act", lambda e: e.activation(out=dst[:, 0:Ln], in_=dst[:, 0:Ln], func=AF.Sin), reads=[dstB], writes=[dstB])
                    src, srcB, K_ = dst, dstB, 64
                S.dma("sp", HID[Ln], src[:, 0:Ln], d, reads=[srcB])

        with Phase(S) as P:
            d = S.dsem(f"hg_{L}")
            w4, w4B = P.sb([64, 4096], F32, "fw4")
            S.dma("sp", w4[:], hy_f_w4[0], d, writes=[w4B])
            absd, absdB = P.sb([128, D], F32, "absd")
            S.dma("sp", absd[:], absd_in.partition_broadcast(128), d, writes=[absdB])
            ones, onesB = P.sb([128, 128], F32, "ones")
            S.dma("sp", ones[:], ones_in, d, writes=[onesB])
            hid, hidB = P.sb([64, 4096], F32, "hid")
            ngt, ngtB = P.sb([128, 32], F32, "ngt")
            dec, decB = P.sb([128, D], F32, "dec")
            hbk, hbkB = P.sb([128, 2, D], F32, "hbk")
            acc, accB = P.sb([128, 2, D], F32, "aacc")
            habs, habsB = P.sb([128, 2, D], F32, "habs")
            nrm, nrmB = P.sb([128, 2, D], F32, "nrm")
            apms = [P.sb([128, 2, D], BF16, f"apm{i}") + (S.dsem(f"apm_{L}_{i}"),) for i in range(2)]
            k = 0
            for Ln in (256, 4096):
                nb = Ln // 128
                S.dma("sp", hid[:, 0:Ln], HID[Ln], d, writes=[hidB])
                S.dma("sp", ngt[:, 0:nb], negt[Ln], d, writes=[ngtB])
                for o in range(2):
                    for blk in range(nb):
                        S.op("act", lambda e: e.activation(out=dec[:], in_=absd[:], func=AF.Exp, scale=ngt[:, blk:blk + 1]), reads=[absdB, ngtB], writes=[decB])
                        for cg in range(4):
                            pp, ppB = nps()
                            S.op("pe", lambda e: e.matmul(pp[:, :], hid[:, blk * 128:(blk + 1) * 128], w4[:, o * 2048 + cg * 512:o * 2048 + (cg + 1) * 512], start=True, stop=True), reads=[hidB, w4B], writes=[ppB])
                            sd_, hf = cg // 2, cg % 2
                            S.op("dve", lambda e: e.tensor_tensor(out=hbk[:, sd_, hf * 512:(hf + 1) * 512], in0=pp[:, :], in1=dec[:, hf * 512:(hf + 1) * 512], op=ALU.mult), reads=[ppB, decB], writes=[hbkB])
                        if blk == 0:
                            S.op("dve", lambda e: e.memset(hbk[0:1, 1, :], 0.0), writes=[hbkB])
                            S.op("act", lambda e: e.activation(out=acc[:], in_=hbk[:], func=AF.Abs), reads=[hbkB], writes=[accB])
                        else:
                            S.op("act", lambda e: e.activation(out=habs[:], in_=hbk[:], func=AF.Abs), reads=[hbkB], writes=[habsB])
                            S.op("dve", lambda e: e.tensor_tensor(out=acc[:], in0=acc[:], in1=habs[:], op=ALU.add), reads=[habsB, accB], writes=[accB])
                        apm, apmB, apmd = apms[k % 2]
                        k += 1
                        S.op("dve", lambda e: e.tensor_tensor(out=apm[:, 0, :], in0=hbk[:, 0, :], in1=hbk[:, 1, :], op=ALU.add), reads=[hbkB], writes=[apmB])
                        S.op("dve", lambda e: e.tensor_tensor(out=apm[:, 1, :], in0=hbk[:, 1, :], in1=hbk[:, 0, :], op=ALU.subtract), reads=[hbkB], writes=[apmB])
                        S.dma("sp", APM[Ln][o, :, :, blk, :].rearrange("s p c -> p s c"), apm[:], apmd, reads=[apmB])
                    for cg in range(4):
                        pp, ppB = nps()
                        sd_, hf = cg // 2, cg % 2
                        S.op("pe", lambda e: e.matmul(pp[:, :], ones[:], acc[:, sd_, hf * 512:(hf + 1) * 512], start=True, stop=True), reads=[onesB, accB], writes=[ppB])
                        S.op("act", lambda e: e.copy(out=nrm[:, sd_, hf * 512:(hf + 1) * 512], in_=pp[:, :]), reads=[ppB], writes=[nrmB])
                    S.op("dve", lambda e: e.tensor_tensor(out=nrm[:, 0, :], in0=nrm[:, 0, :], in1=nrm[:, 1, :], op=ALU.add), reads=[nrmB], writes=[nrmB])
                    S.op("dve", lambda e: e.reciprocal(out=nrm[:, 1, :], in_=nrm[:, 0, :]), reads=[nrmB], writes=[nrmB])
                    S.dma("sp", RIN[Ln][o:o + 1, :], nrm[0:1, 1, :], d, reads=[nrmB])

        def fwd_dft(P, Ln, R1, R1B, R2, R2B, emit, tag):
            nb, nfc = Ln // 128, Ln // 128 + 1
            FC_, FS_ = FCm[Ln]
            fts = [P.sb([128, nb, 128], BF16, f"ft{tag}{i}") + (S.dsem(f"ft_{tag}_{i}"),) for i in range(4)]
            def _ld(fc):
                a, aB, ad = fts[(2 * fc) % 4]
                b, bB, bd = fts[(2 * fc + 1) % 4]
                S.dma("sp", a[:], FC_[fc], ad, writes=[aB])
                S.dma("sp", b[:], FS_[fc], bd, writes=[bB])
            _ld(0)
            for fc in range(nfc):
                fct, fcB, fcd = fts[(2 * fc) % 4]
                fst, fsB, fsd = fts[(2 * fc + 1) % 4]
                if fc + 1 < nfc:
                    _ld(fc + 1)
                for half in range(2):
                    pc, pcB = nps()
                    for blk in range(nb):
                        S.op("pe", lambda e: e.matmul(pc[:, :], fct[:, blk, :], R1[:, blk, half * 512:(half + 1) * 512], start=(blk == 0), stop=(blk == nb - 1)),
                             reads=[fcB, R1B], writes=[pcB], inc=(blk == nb - 1))
                    pS, pSB = nps()
                    for blk in range(nb):
                        S.op("pe", lambda e: e.matmul(pS[:, :], fst[:, blk, :], R2[:, blk, half * 512:(half + 1) * 512], start=(blk == 0), stop=(blk == nb - 1)),
                             reads=[fsB, R2B], writes=[pSB], inc=(blk == nb - 1))
                    emit(fc, half, pc, pcB, pS, pSB)

        for Ln in (256, 4096):
            nb = Ln // 128
            for o in range(2):
                with Phase(S) as P:
                    d = S.dsem(f"hk_{L}_{Ln}_{o}")
                    Ap, ApB = P.sb([128, nb, D], BF16, "Ap")
                    Am, AmB = P.sb([128, nb, D], BF16, "Am")
                    S.dma("sp", Ap[:], APM[Ln][o, 0], d, writes=[ApB])
                    S.dma("sp", Am[:], APM[Ln][o, 1], d, writes=[AmB])
                    rin, rinB = P.sb([128, D], F32, "rin")
                    S.dma("sp", rin[:], RIN[Ln][o, :].partition_broadcast(128), d, writes=[rinB])
                    kos = [P.sb([128, 2, 512], F32, f"ko{i}") + (S.dsem(f"ko_{L}_{Ln}_{o}_{i}"),) for i in range(2)]
                    cnt = [0]

                    def emit(fc, half, pc, pcB, pS, pSB):
                        ko, koB, kod = kos[cnt[0] % 2]
                        cnt[0] += 1
                        hs = slice(half * 512, (half + 1) * 512)
                        S.op("dve", lambda e: e.tensor_tensor(out=ko[:, 0, :], in0=pc[:, :], in1=rin[:, hs], op=ALU.mult), reads=[pcB, rinB], writes=[koB])
                        S.op("dve", lambda e: e.tensor_tensor(out=ko[:, 1, :], in0=pS[:, :], in1=rin[:, hs], op=ALU.mult), reads=[pSB, rinB], writes=[koB])
                        S.dma("act", KF[Ln][o, :, fc * 128:(fc + 1) * 128, hs].rearrange("r p c -> p r c"), ko[:], kod, reads=[koB])

                    fwd_dft(P, Ln, Ap, ApB, Am, AmB, emit, f"k{Ln}{o}")

        for o in range(2):
            for Ln in (256, 4096):
                nb, nfc = Ln // 128, Ln // 128 + 1
                tk0 = TOK0[Ln]
                src = UT[2] if o == 0 else Z1
                with Phase(S) as P:
                    d = S.dsem(f"hy_{L}_{o}_{Ln}")
                    R, RB_ = P.sb([128, nb, D], BF16, "R")
                    S.dma("pool", R[:], src[tk0:tk0 + Ln, :].rearrange("(b p) c -> p b c", p=128), d, writes=[RB_])
                    kts = [P.sb([128, 2, 512], F32, f"kt{i}") + (S.dsem(f"kt_{L}_{o}_{Ln}_{i}"),) for i in range(2)]
                    yos = [P.sb([128, 2, 512], BF16, f"yo{i}") + (S.dsem(f"yo_{L}_{o}_{Ln}_{i}"),) for i in range(2)]
                    t1, t1B = P.sb([128, 512], F32, "yt1")
                    t2, t2B = P.sb([128, 512], F32, "yt2")
                    cnt = [0]

                    def emit(fc, half, pc, pcB, pS, pSB):
                        kt, ktB, ktd = kts[cnt[0] % 2]
                        yo, yoB, yod = yos[cnt[0] % 2]
                        cnt[0] += 1
                        hs = slice(half * 512, (half + 1) * 512)
                        S.dma("sp", kt[:], KF[Ln][o, :, fc * 128:(fc + 1) * 128, hs].rearrange("r p c -> p r c"), ktd, writes=[ktB])
                        S.op("dve", lambda e: e.tensor_tensor(out=t1[:], in0=pc[:, :], in1=kt[:, 0, :], op=ALU.mult), reads=[pcB, ktB], writes=[t1B])
                        S.op("dve", lambda e: e.tensor_tensor(out=t2[:], in0=pS[:, :], in1=kt[:, 1, :], op=ALU.mult), reads=[pSB, ktB], writes=[t2B])
                        S.op("dve", lambda e: e.tensor_tensor(out=yo[:, 0, :], in0=t1[:], in1=t2[:], op=ALU.add), reads=[t1B, t2B], writes=[yoB])
                        S.op("dve", lambda e: e.tensor_tensor(out=t1[:], in0=pc[:, :], in1=kt[:, 1, :], op=ALU.mult), reads=[pcB, ktB], writes=[t1B])
                        S.op("dve", lambda e: e.tensor_tensor(out=t2[:], in0=pS[:, :], in1=kt[:, 0, :], op=ALU.mult), reads=[pSB, ktB], writes=[t2B])
                        S.op("dve", lambda e: e.tensor_tensor(out=yo[:, 1, :], in0=t1[:], in1=t2[:], op=ALU.subtract), reads=[t1B, t2B], writes=[yoB])
                        S.dma("act", YS[Ln][:, fc * 128:(fc + 1) * 128, hs].rearrange("r p c -> p r c"), yo[:], yod, reads=[yoB])

                    fwd_dft(P, Ln, R, RB_, R, RB_, emit, f"y{Ln}{o}")

                with Phase(S) as P:
                    d = S.dsem(f"hi_{L}_{o}_{Ln}")
                    Yr, YrB = P.sb([128, nfc, D], BF16, "Yr")
                    Yi, YiB = P.sb([128, nfc, D], BF16, "Yi")
                    S.dma("sp", Yr[:], YS[Ln][0].rearrange("(f p) c -> p f c", p=128), d, writes=[YrB])
                    S.dma("sp", Yi[:], YS[Ln][1].rearrange("(f p) c -> p f c", p=128), d, writes=[YiB])
                    skp, skpB = P.sb([128, D], F32, "skp")
                    S.dma("sp", skp[:], hy_skip[0, o, :].partition_broadcast(128), d, writes=[skpB])
                    GC_, GS_ = GCm[Ln]
                    gts = [P.sb([128, nfc, 128], BF16, f"gt{i}") + (S.dsem(f"gt_{L}_{o}_{Ln}_{i}"),) for i in range(4)]
                    ins = [P.sb([128, D], F32, f"in{i}") + (S.dsem(f"in_{L}_{o}_{Ln}_{i}"),) for i in range(2)]
                    ggs = [P.sb([128, D], F32, f"gg{i}") + (S.dsem(f"gg_{L}_{o}_{Ln}_{i}"),) for i in range(2)]
                    zos = [P.sb([128, D], F32, f"zo{i}") + (S.dsem(f"zo_{L}_{o}_{Ln}_{i}"),) for i in range(2)]
                    osts = [P.sb([128, 8, 128], BF16, f"hos{i}") + (S.dsem(f"hos_{L}_{o}_{Ln}_{i}"),) for i in range(2)]
                    def _ldi(tb):
                        a, aB, ad = gts[(2 * tb) % 4]
                        b, bB, bd = gts[(2 * tb + 1) % 4]
                        S.dma("sp", a[:], GC_[tb], ad, writes=[aB])
                        S.dma("sp", b[:], GS_[tb], bd, writes=[bB])
                        i_, iB, idd = ins[tb % 2]
                        g_, gB_, gdd = ggs[tb % 2]
                        r0_ = tk0 + tb * 128
                        S.dma("sp", i_[:], src[r0_:r0_ + 128, :], idd, writes=[iB])
                        S.dma("sp", g_[:], UT[o, r0_:r0_ + 128, :], gdd, writes=[gB_])
                    _ldi(0)
                    for tb in range(nb):
                        gct, gcB, gcd = gts[(2 * tb) % 4]
                        gst, gsB, gsd = gts[(2 * tb + 1) % 4]
                        it_, itB, itd = ins[tb % 2]
                        gg, ggB, ggd = ggs[tb % 2]
                        zo, zoB, zod = zos[tb % 2]
                        r0 = tk0 + tb * 128
                        if tb + 1 < nb:
                            _ldi(tb + 1)
                        S.op("dve", lambda e: e.tensor_tensor(out=zo[:], in0=it_[:], in1=skp[:], op=ALU.mult), reads=[itB, skpB], writes=[zoB])
                        for half in range(2):
                            hs = slice(half * 512, (half + 1) * 512)
                            py, pyB = nps()
                            for fc in range(nfc):
                                S.op("pe", lambda e: e.matmul(py[:, :], gct[:, fc, :], Yr[:, fc, hs], start=(fc == 0), stop=False), reads=[gcB, YrB], writes=[pyB], inc=False)
                            for fc in range(nfc):
                                S.op("pe", lambda e: e.matmul(py[:, :], gst[:, fc, :], Yi[:, fc, hs], start=False, stop=(fc == nfc - 1)), reads=[gsB, YiB], writes=[pyB], inc=(fc == nfc - 1))
                            S.op("dve", lambda e: e.tensor_tensor(out=zo[:, hs], in0=zo[:, hs], in1=py[:, :], op=ALU.add), reads=[zoB, pyB], writes=[zoB])
                        S.op("dve", lambda e: e.tensor_tensor(out=zo[:], in0=zo[:], in1=gg[:], op=ALU.mult), reads=[zoB, ggB], writes=[zoB])
                        if o == 0:
                            S.dma("act", Z1[r0:r0 + 128, :], zo[:], zod, reads=[zoB])
                        else:
                            os_, osB, osd = osts[tb % 2]
                            for half in range(2):
                                pt, pB = nps()
                                for q in range(4):
                                    kc = half * 4 + q
                                    S.op("pe", lambda e: e.transpose(pt[:, q * 128:(q + 1) * 128], zo[:, kc * 128:(kc + 1) * 128], ident[:]), reads=[zoB, identB], writes=[pB], inc=(q == 3))
                                S.op("act", lambda e: e.copy(out=os_[:, half * 4:half * 4 + 4, :], in_=pt[:, :].rearrange("p (a b) -> p a b", a=4)), reads=[pB], writes=[osB])
                            S.dma("act", OT[:, :, r0:r0 + 128].rearrange("j p t -> p j t"), os_[:], osd, reads=[osB], writes=otbuf)
        phase_outproj_norm2(L, hy_w_out[0], hy_b_out[0, :], last, None)

    for L, part in segs:
        last = (L == 3)
        mk = L % 3
        if flags.get("skip_mixer") or part == "ffn":
            pass
        elif mk == 0:
            mixer_rglru(L, L // 3, last)
        elif mk == 1:
            mixer_swa(L, last)
        else:
            mixer_hyena(L, last)
        if not flags.get("skip_ffn") and part != "mix":
            phase_ffn(L, L // 2 if L % 2 == 0 else None, L // 2 if L % 2 == 1 else None, last)

    with Phase(S) as P:
        fd = S.dsem("fin")
        S.dma("sp", xs_out, XS[:, :], fd, reads=xsbuf)
        gfin, gfinB = P.sb([128, D], F32, "gfin")
        S.dma("sp", gfin[:], norm_final.partition_broadcast(128), fd, writes=[gfinB])
        xsl = [P.sb([128, D], F32, f"x{i}") + (S.dsem(f"xf_{i}"),) for i in range(3)]
        junk, junkB = P.sb([128, D], BF16, "junk")
        st = P.sb([128, 4], F32, "st")
        issued = set()

        def ensure_x(blk):
            if blk < NBLK and blk not in issued:
                issued.add(blk)
                xt, xB, xd = xsl[blk % 3]
                S.dma("sp", xt[:], XS[blk * 128:(blk + 1) * 128, :], xd, reads=[xsbuf[blk]], writes=[xB])
        for blk in range(2, NBLK):
            ensure_x(blk)
            ensure_x(blk + 1)
            xt, xB, xd = xsl[blk % 3]
            rms_rstd(P, xt, xB, junk, junkB, st)
            S.op("dve", lambda e, xt=xt: e.scalar_tensor_tensor(out=xt[:], in0=xt[:], scalar=st[0][:, 2:3], in1=gfin[:], op0=ALU.mult, op1=ALU.mult),
                 reads=[xB, st[1], gfinB], writes=[xB])
            S.dma("sp", out[(blk - 2) * 128:(blk - 1) * 128, :], xt[:], xd, reads=[xB])
    G.__exit__()
    es.close()
    return nc, S


_COMMON = ["xs_in", "c2T", "ada_w", "ada_b", "norm_mix", "norm_ffn", "norm_final", "ident", "identb"]
_LRU = ["lru_w_in", "lru_conv_wT", "lru_conv_bT", "lru_w_a", "lru_b_aT", "lru_w_x", "lru_b_xT", "lru_lamT", "lru_w_out"]
_SWA = ["attn_w_qkv", "attn_w_qkp", "attn_sinks", "attn_w_o", "maskb", "ropec", "ropes"]
_HY = ["hy_w_in", "hy_b_inT", "hy_conv_wT", "hy_conv_bT", "hy_f_w1", "hy_f_b1", "hy_f_w2", "hy_f_b2", "hy_f_w3", "hy_f_b3", "hy_f_freq", "hy_f_w4",
       "hy_skip", "hy_w_out", "hy_b_out", "ones", "absd", "zT256", "zT4096", "negt256", "negt4096",
       "FC256", "FS256", "GC256", "GS256", "FC4096", "FS4096", "GC4096", "GS4096"]
_DENSE = ["ffn_w_gate", "ffn_w_up", "ffn_w_down"]
_MOE = ["moe_router", "moe_w_gate", "moe_w_up", "moe_w_down"]


def _needed(segs):
    n = set(_COMMON)
    for sg in segs:
        L, part = (sg, "all") if isinstance(sg, int) else sg
        if part != "ffn":
            n |= set((_LRU, _SWA, _HY)[L % 3])
            if L % 2 == 1:
                n.add("moe_router")
        if part != "mix":
            n |= set(_DENSE if L % 2 == 0 else _MOE)
        if part == "ffn":
            n |= {"h2t_in", "gts_in"}
    return n


def _consts():
    c = {}
    c["ident"] = np.eye(128, dtype=np.float32)
    c["identb"] = np.eye(128, dtype=np.float32).astype(ml_dtypes.bfloat16)
    q = np.arange(128)[:, None]
    jk = np.arange(128)[None, :]
    m = np.zeros((128, 384), np.float32)
    m[:, 0:128] = np.where(jk >= q, 0.0, -1e30)
    m[:, 256:384] = np.where(jk <= q, 0.0, -1e30)
    c["maskb"] = m
    qd = 16
    inv = (10000.0 ** (-np.arange(qd, dtype=np.float32) / qd)).astype(np.float32)
    rows = SEQ // 64
    row = np.repeat(np.arange(rows, dtype=np.float32), 64)
    col = np.tile(np.arange(64, dtype=np.float32), rows)
    ang = np.concatenate([row[:, None] * inv, col[:, None] * inv], axis=-1).astype(np.float32)
    cos, sin = np.cos(ang), np.sin(ang)
    rc = np.ones((64, T), np.float32)
    rs = np.zeros((64, T), np.float32)
    for d in range(64):
        half = 0 if d < 32 else 16
        rc[d, CTX:] = cos[:, half + d % 16]
        sgn = -1.0 if (d % 32) < 16 else 1.0
        rs[d, CTX:] = sgn * sin[:, half + d % 16]
    c["ropec"], c["ropes"] = rc, rs
    c["ones"] = np.ones((128, 128), np.float32)
    deltas = np.linspace(math.log(1e-2) / 1.5, math.log(1e-2) / 0.3, D, dtype=np.float32)
    c["absd"] = np.abs(deltas).astype(np.float32)
    for Ln in (256, 4096):
        nb, nfc = Ln // 128, Ln // 128 + 1
        t = np.linspace(0.0, 1.0, Ln, dtype=np.float32)[:, None]
        omega = ((2.0 * math.pi / Ln) * np.arange(Ln, dtype=np.float32))[:, None].astype(np.float32)
        bands = np.linspace(1e-4, 15, 16, dtype=np.float32)[None, :]
        z = np.concatenate([t, np.cos(bands * omega), -np.sin(bands * omega)], axis=-1).astype(np.float32)
        c[f"zT{Ln}"] = np.ascontiguousarray(z.T)
        c[f"negt{Ln}"] = np.ascontiguousarray((-t[:, 0]).reshape(nb, 128).T)
        N2 = 2 * Ln
        tt = np.arange(Ln, dtype=np.int64)
        kk = np.arange(nfc * 128, dtype=np.int64)
        ang = (2.0 * np.pi / N2) * ((tt[:, None] * kk[None, :]) % N2).astype(np.float64)
        valid = (kk <= Ln).astype(np.float64)[None, :]
        Cm = np.cos(ang) * valid
        Sm = np.sin(ang) * valid
        c[f"FC{Ln}"] = np.ascontiguousarray(Cm.reshape(nb, 128, nfc, 128).transpose(2, 1, 0, 3)).astype(ml_dtypes.bfloat16)
        c[f"FS{Ln}"] = np.ascontiguousarray(Sm.reshape(nb, 128, nfc, 128).transpose(2, 1, 0, 3)).astype(ml_dtypes.bfloat16)
        wk = np.where((kk == 0) | (kk == Ln), 1.0, 2.0) * (kk <= Ln) / N2
        Gc = (Cm * wk[None, :]).T
        Gs = (-Sm * wk[None, :]).T
        c[f"GC{Ln}"] = np.ascontiguousarray(Gc.reshape(nfc, 128, nb, 128).transpose(2, 1, 0, 3)).astype(ml_dtypes.bfloat16)
        c[f"GS{Ln}"] = np.ascontiguousarray(Gs.reshape(nfc, 128, nb, 128).transpose(2, 1, 0, 3)).astype(ml_dtypes.bfloat16)
    return c


_CONSTS = None


def _prep(inputs, b):
    global _CONSTS
    if _CONSTS is None:
        _CONSTS = _consts()
    f = lambda a: np.ascontiguousarray(a, dtype=np.float32)
    m = {}
    m["xs_in"] = f(np.concatenate([inputs["ctx"][b], inputs["x"][b]], axis=0))
    c2 = np.stack([inputs["c"][b], inputs["c_ctx"]], axis=-1)
    m["c2T"] = f(c2.reshape(8, 128, 2).transpose(1, 0, 2))
    for k in ("ada_w", "ada_b", "norm_mix", "norm_ffn", "norm_final", "lru_w_in", "lru_w_a", "lru_w_x", "lru_w_out", "attn_w_qkv", "attn_sinks", "attn_w_o",
              "hy_w_in", "hy_f_w1", "hy_f_w2", "hy_f_w3", "hy_f_w4", "hy_skip", "hy_w_out", "hy_b_out", "ffn_w_gate", "ffn_w_up", "ffn_w_down",
              "moe_router", "moe_w_gate", "moe_w_up", "moe_w_down"):
        m[k] = f(inputs[k])
    m["lru_conv_wT"] = f(inputs["lru_conv_w"].reshape(2, 4, 8, 128).transpose(0, 3, 2, 1))
    m["lru_conv_bT"] = f(inputs["lru_conv_b"].reshape(2, 8, 128).transpose(0, 2, 1))
    for k in ("lru_b_a", "lru_b_x"):
        m[k + "T"] = f(inputs[k].reshape(2, 2, 8, 128).transpose(0, 1, 3, 2))
    m["lru_lamT"] = f(inputs["lru_lambda"].reshape(2, 2, 8, 128).transpose(0, 1, 3, 2))
    perm = np.concatenate([np.arange(16, 32), np.arange(0, 16), np.arange(48, 64), np.arange(32, 48)])
    cols = np.concatenate([h * 64 + perm for h in range(20)])
    m["attn_w_qkp"] = f(inputs["attn_w_qkv"][:, :, cols])
    m["hy_b_inT"] = f(inputs["hy_b_in"].reshape(1, 24, 128).transpose(0, 2, 1))
    m["hy_conv_wT"] = f(inputs["hy_conv_w"].reshape(1, 3, 24, 128).transpose(0, 3, 2, 1))
    m["hy_conv_bT"] = f(inputs["hy_conv_b"].reshape(1, 24, 128).transpose(0, 2, 1))
    for k in ("hy_f_b1", "hy_f_b2", "hy_f_b3", "hy_f_freq"):
        m[k] = f(inputs[k].reshape(1, 64, 1))
    m.update(_CONSTS)
    return m


def kernel(**inputs):
    maps = [_prep(inputs, b) for b in range(8)]
    outs = None
    for seg in SEGMENTS:
        nc, S = build(seg)
        need = _needed(seg)
        in_maps = [{k: v for k, v in m.items() if k in need} for m in maps]
        res = run_bass_kernel_spmd(nc, in_maps, core_ids=list(range(8)))
        for b in range(8):
            r = res.results[b]
            maps[b]["xs_in"] = np.asarray(r["xs_out"], dtype=np.float32)
            if "h2t_out" in r:
                maps[b]["h2t_in"] = np.asarray(r["h2t_out"])
                maps[b]["gts_in"] = np.asarray(r["gts_out"], dtype=np.float32)
        outs = [np.asarray(r["out"], dtype=np.float32) for r in res.results]
    return np.stack(outs, axis=0)


SEGMENTS = [[(0, "all"), (1, "all"), (2, "all"), (3, "all")]]
```
